# Optimizing a Trainium2 kernel written in Bass

```python
import math
import jax, jax.numpy as jnp
from jax import lax
import numpy as np

D_MODEL = 1024
BATCH = 2
SEQ = 16384
DEPTH = 4

GRID_W = 64
Q_BLOCK = 128
N_BRANCHES = 4
BRANCH_WIDTH = 256
A_Q_HEADS = 4
A_KV_HEADS = 2
A_HEAD_DIM = 64
ROPE_THETA = 10000.0
SSM_HEADS = 4
SSM_HEAD_DIM = 64
SSM_GROUPS = 2
SSM_STATE = 128
SSM_CONV = 5
SSM_CHUNK = 128
DIFF_HEADS = 4
DIFF_QK_DIM = 32
DIFF_V_DIM = 64
REL_BUCKETS = 32
REL_MAX_DIST = 128
SGU_GROUPS = 4
SGU_GROUP_DIM = 64
SGU_CHUNK = 128
N_EXPERTS = 16
EC_CAPACITY = 2
D_FF_EXPERT = 1024
PLE_DIM = 256
EPS = 1e-6

IN_SIZES = (
    A_Q_HEADS * A_HEAD_DIM, A_KV_HEADS * A_HEAD_DIM, A_KV_HEADS * A_HEAD_DIM,
    SSM_HEADS * SSM_HEAD_DIM, SSM_HEADS * SSM_HEAD_DIM,
    SSM_GROUPS * SSM_STATE, SSM_GROUPS * SSM_STATE, SSM_HEADS, SSM_HEADS,
    DIFF_HEADS * 2 * DIFF_QK_DIM, DIFF_HEADS * 2 * DIFF_QK_DIM, DIFF_HEADS * DIFF_V_DIM,
    2 * SGU_GROUPS * SGU_GROUP_DIM,
)
IN_WIDTH = sum(IN_SIZES)
CONV_CH = SSM_HEADS * SSM_HEAD_DIM + 2 * SSM_GROUPS * SSM_STATE

kernel_name = 'hybrid_gated_parallel_encoder'


def rms_norm(x, g):
    xf = x.astype(jnp.float32)
    y = xf * lax.rsqrt(jnp.mean(xf * xf, axis=-1, keepdims=True) + EPS)
    return (y * g.astype(jnp.float32)).astype(x.dtype)


def rope_1d(xh, pos):
    n = xh.shape[-1]
    freqs = ROPE_THETA ** (-jnp.arange(0, n, 2, dtype=jnp.float32) / n)
    ang = pos.astype(jnp.float32)[:, None] * freqs[None, :]
    cos = jnp.cos(ang)[None, :, None, :]
    sin = jnp.sin(ang)[None, :, None, :]
    x1 = xh[..., : n // 2].astype(jnp.float32)
    x2 = xh[..., n // 2:].astype(jnp.float32)
    return jnp.concatenate([x1 * cos - x2 * sin, x1 * sin + x2 * cos], axis=-1).astype(xh.dtype)


def axial_rope(x, row, col):
    half = x.shape[-1] // 2
    return jnp.concatenate([rope_1d(x[..., :half], row), rope_1d(x[..., half:], col)], axis=-1)


def gqa_block_attention(q, k, v):
    b, s, hq, d = q.shape
    hkv = k.shape[2]
    nblk = s // Q_BLOCK
    scale = d ** -0.5
    qb = jnp.moveaxis(q.reshape(b, nblk, Q_BLOCK, hkv, hq // hkv, d), 1, 0)

    def one_block(qi):
        logits = jnp.einsum('bqgrd,bkgd->bgrqk', qi, k).astype(jnp.float32) * scale
        probs = jax.nn.softmax(logits, axis=-1).astype(v.dtype)
        return jnp.einsum('bgrqk,bkgd->bqgrd', probs, v)

    out = lax.map(one_block, qb)
    return jnp.moveaxis(out, 0, 1).reshape(b, s, hq * d)


def t5_bucket(rel):
    nb = REL_BUCKETS // 2
    max_exact = nb // 2
    ret = jnp.where(rel > 0, nb, 0)
    r = jnp.abs(rel)
    rf = jnp.maximum(r, 1).astype(jnp.float32)
    large = max_exact + (jnp.log(rf / max_exact) / math.log(REL_MAX_DIST / max_exact)
                         * (nb - max_exact)).astype(jnp.int32)
    large = jnp.minimum(large, nb - 1)
    return ret + jnp.where(r < max_exact, r, large)


def diff_block_attention(q1, q2, k1, k2, v, lam, rel_bias):
    b, s, h, d = q1.shape
    nblk = s // Q_BLOCK
    scale = d ** -0.5
    kpos = jnp.arange(s, dtype=jnp.int32)
    starts = jnp.arange(nblk, dtype=jnp.int32) * Q_BLOCK

    def split_blocks(t):
        return jnp.moveaxis(t.reshape(b, nblk, Q_BLOCK, h, d), 1, 0)

    def one_block(args):
        q1i, q2i, start = args
        qpos = start + jnp.arange(Q_BLOCK, dtype=jnp.int32)
        bias = jnp.transpose(rel_bias[t5_bucket(kpos[None, :] - qpos[:, None])], (2, 0, 1)).astype(jnp.float32)
        p1 = jax.nn.softmax(jnp.einsum('bqhd,bkhd->bhqk', q1i, k1).astype(jnp.float32) * scale + bias, axis=-1)
        p2 = jax.nn.softmax(jnp.einsum('bqhd,bkhd->bhqk', q2i, k2).astype(jnp.float32) * scale + bias, axis=-1)
        w = (p1 - lam * p2).astype(v.dtype)
        return jnp.einsum('bhqk,bkhd->bqhd', w, v)

    out = lax.map(one_block, (split_blocks(q1), split_blocks(q2), starts))
    return jnp.moveaxis(out, 0, 1).reshape(b, s, h, v.shape[-1])


def depthwise_conv_centred(x, w, bias):
    k, c = w.shape
    y = lax.conv_general_dilated(x, w[:, None, :].astype(x.dtype), window_strides=(1,),
                                 padding=[((k - 1) // 2, k // 2)],
                                 dimension_numbers=('NWC', 'WIO', 'NWC'), feature_group_count=c)
    return y + bias.astype(x.dtype)


def ssd_chunked(x, dt, a, bm, cm):
    dtype = x.dtype
    f32 = jnp.float32
    b, s, h, pdim = x.shape
    nc = s // SSM_CHUNK
    rep = h // bm.shape[2]
    xc = (x.astype(f32) * dt[..., None]).reshape(b, nc, SSM_CHUNK, h, pdim)
    bc = jnp.repeat(bm.astype(f32), rep, axis=2).reshape(b, nc, SSM_CHUNK, h, -1)
    cc = jnp.repeat(cm.astype(f32), rep, axis=2).reshape(b, nc, SSM_CHUNK, h, -1)
    acs = jnp.cumsum((dt * a[None, None, :]).reshape(b, nc, SSM_CHUNK, h), axis=2)
    causal = jnp.tril(jnp.ones((SSM_CHUNK, SSM_CHUNK), dtype=bool))
    seg = acs[:, :, :, None, :] - acs[:, :, None, :, :]
    decay_in = jnp.exp(jnp.where(causal[None, None, :, :, None], seg, -jnp.inf))
    scores = jnp.einsum('bclhn,bcshn->bclsh', cc, bc) * decay_in
    y_diag = jnp.einsum('bclsh,bcshp->bclhp', scores, xc)
    decay_to_end = jnp.exp(acs[:, :, -1:, :] - acs)
    chunk_states = jnp.einsum('bclhn,bclh,bclhp->bchpn', bc, decay_to_end, xc)
    chunk_decay = jnp.exp(acs[:, :, -1, :])

    def carry_state(state, inp):
        cs, cd = inp
        return state * cd[:, :, None, None] + cs, state

    init = jnp.zeros((b, h, pdim, bc.shape[-1]), f32)
    _, prev = lax.scan(carry_state, init, (jnp.moveaxis(chunk_states, 1, 0), jnp.moveaxis(chunk_decay, 1, 0)))
    prev = jnp.moveaxis(prev, 0, 1)
    y_off = jnp.einsum('bclhn,bchpn,bclh->bclhp', cc, prev, jnp.exp(acs))
    return (y_diag + y_off).reshape(b, s, h, pdim).astype(dtype)


def bidir_ssd(z, xs, bm, cm, dt_f, dt_b, conv_w, conv_b, dtb_f, dtb_b, alog_f, alog_b, d_skip, g_norm):
    f32 = jnp.float32
    b, s, _ = xs.shape
    d_x = SSM_HEADS * SSM_HEAD_DIM
    d_bc = SSM_GROUPS * SSM_STATE
    xbc = jax.nn.silu(depthwise_conv_centred(jnp.concatenate([xs, bm, cm], axis=-1), conv_w, conv_b))
    xh = xbc[..., :d_x].reshape(b, s, SSM_HEADS, SSM_HEAD_DIM)
    bg = xbc[..., d_x:d_x + d_bc].reshape(b, s, SSM_GROUPS, SSM_STATE)
    cg = xbc[..., d_x + d_bc:].reshape(b, s, SSM_GROUPS, SSM_STATE)
    delta_f = jax.nn.softplus(dt_f.astype(f32) + dtb_f.astype(f32))
    delta_b = jax.nn.softplus(dt_b.astype(f32) + dtb_b.astype(f32))
    y_f = ssd_chunked(xh, delta_f, -jnp.exp(alog_f.astype(f32)), bg, cg)
    y_b = jnp.flip(ssd_chunked(jnp.flip(xh, 1), jnp.flip(delta_b, 1), -jnp.exp(alog_b.astype(f32)),
                               jnp.flip(bg, 1), jnp.flip(cg, 1)), 1)
    y = y_f + y_b + d_skip[:, None].astype(xh.dtype) * xh
    y = y.reshape(b, s, d_x) * jax.nn.silu(z)
    return rms_norm(y, g_norm)


def spatial_gating(uv, g_norm, w_s, b_s):
    b, s, _ = uv.shape
    u, v = jnp.split(jax.nn.gelu(uv), 2, axis=-1)
    v = rms_norm(v, g_norm).reshape(b, s // SGU_CHUNK, SGU_CHUNK, SGU_GROUPS, SGU_GROUP_DIM)
    sv = jnp.einsum('gts,bcsgd->bctgd', w_s.astype(v.dtype), v) + b_s.T[None, None, :, :, None].astype(v.dtype)
    return u * sv.reshape(b, s, SGU_GROUPS * SGU_GROUP_DIM)


def expert_choice_moe(h, w_router, w_gate_e, w_up_e, w_down_e):
    b, s, _ = h.shape
    cap = EC_CAPACITY * s // N_EXPERTS
    affinity = jax.nn.softmax((h @ w_router).astype(jnp.float32), axis=-1)
    top_aff, idx = lax.top_k(jnp.swapaxes(affinity, 1, 2), cap)
    bidx = jnp.arange(b)[:, None, None]
    xe = h[bidx, idx]
    hid = jax.nn.silu(jnp.einsum('becd,edf->becf', xe, w_gate_e)) * jnp.einsum('becd,edf->becf', xe, w_up_e)
    ye = jnp.einsum('becf,efd->becd', hid, w_down_e)
    return jnp.zeros_like(h).at[bidx, idx].add(ye * top_aff[..., None].astype(ye.dtype))


def setup_inputs(seed: int = 0) -> dict:
    key = jax.random.key(seed)
    keys = list(jax.random.split(key, 40))

    def nrm(shape, scale):
        return jax.random.normal(keys.pop(), shape, jnp.float32) * scale

    def gain(shape):
        return 1.0 + nrm(shape, 0.05)

    def dt_bias(shape):
        u = jax.random.uniform(keys.pop(), shape, jnp.float32)
        dt = jnp.exp(math.log(1e-3) + u * (math.log(1e-1) - math.log(1e-3)))
        return dt + jnp.log(-jnp.expm1(-dt))

    def a_log(shape):
        return jnp.log(jax.random.uniform(keys.pop(), shape, jnp.float32, 1.0, 16.0))

    L = DEPTH
    return {
        'x': nrm((BATCH, SEQ, D_MODEL), 1.0),
        'p': nrm((DEPTH, BATCH, SEQ, PLE_DIM), 1.0),
        'rel_bias': nrm((REL_BUCKETS, DIFF_HEADS), 0.5),
        'g_mix': gain((L, D_MODEL)),
        'w_in': nrm((L, D_MODEL, IN_WIDTH), D_MODEL ** -0.5),
        'g_qnorm': gain((L, A_HEAD_DIM)),
        'g_knorm': gain((L, A_HEAD_DIM)),
        'conv_w': nrm((L, SSM_CONV, CONV_CH), SSM_CONV ** -0.5),
        'conv_b': nrm((L, CONV_CH), 0.02),
        'dt_bias_f': dt_bias((L, SSM_HEADS)),
        'dt_bias_b': dt_bias((L, SSM_HEADS)),
        'a_log_f': a_log((L, SSM_HEADS)),
        'a_log_b': a_log((L, SSM_HEADS)),
        'd_skip': gain((L, SSM_HEADS)),
        'g_ssm': gain((L, SSM_HEADS * SSM_HEAD_DIM)),
        'lambda_q1': nrm((L, DIFF_QK_DIM), 0.1),
        'lambda_k1': nrm((L, DIFF_QK_DIM), 0.1),
        'lambda_q2': nrm((L, DIFF_QK_DIM), 0.1),
        'lambda_k2': nrm((L, DIFF_QK_DIM), 0.1),
        'g_diff': gain((L, DIFF_V_DIM)),
        'g_sgu': gain((L, SGU_GROUPS * SGU_GROUP_DIM)),
        'w_spatial': nrm((L, SGU_GROUPS, SGU_CHUNK, SGU_CHUNK), SGU_CHUNK ** -0.5),
        'b_spatial': gain((L, SGU_GROUPS, SGU_CHUNK)),
        'w_branch': nrm((L, N_BRANCHES, BRANCH_WIDTH, D_MODEL), BRANCH_WIDTH ** -0.5),
        'w_branch_gate': nrm((L, D_MODEL, N_BRANCHES * D_MODEL), D_MODEL ** -0.5),
        'w_out': nrm((L, D_MODEL, D_MODEL), D_MODEL ** -0.5),
        'g_moe': gain((L, D_MODEL)),
        'w_router': nrm((L, D_MODEL, N_EXPERTS), D_MODEL ** -0.5),
        'w_exp_gate': nrm((L, N_EXPERTS, D_MODEL, D_FF_EXPERT), D_MODEL ** -0.5),
        'w_exp_up': nrm((L, N_EXPERTS, D_MODEL, D_FF_EXPERT), D_MODEL ** -0.5),
        'w_exp_down': nrm((L, N_EXPERTS, D_FF_EXPERT, D_MODEL), D_FF_EXPERT ** -0.5),
        'g_ple': gain((L, D_MODEL)),
        'w_ple_gate': nrm((L, D_MODEL, D_MODEL), D_MODEL ** -0.5),
        'w_ple': nrm((L, PLE_DIM, D_MODEL), PLE_DIM ** -0.5),
        'g_final': gain((D_MODEL,)),
    }


def reference(x, p, rel_bias, g_mix, w_in, g_qnorm, g_knorm, conv_w, conv_b, dt_bias_f, dt_bias_b,
              a_log_f, a_log_b, d_skip, g_ssm, lambda_q1, lambda_k1, lambda_q2, lambda_k2, g_diff,
              g_sgu, w_spatial, b_spatial, w_branch, w_branch_gate, w_out, g_moe, w_router,
              w_exp_gate, w_exp_up, w_exp_down, g_ple, w_ple_gate, w_ple, g_final):
    f32 = jnp.float32
    b, s, _ = x.shape
    rows = s // GRID_W
    row = jnp.repeat(jnp.arange(rows, dtype=jnp.int32), GRID_W)
    col = jnp.tile(jnp.arange(GRID_W, dtype=jnp.int32), rows)
    offsets = [int(o) for o in np.cumsum(IN_SIZES)[:-1]]

    for i in range(DEPTH):
        h = rms_norm(x, g_mix[i])
        (a_q, a_k, a_v, s_z, s_x, s_b, s_c, s_dtf, s_dtb,
         c_q, c_k, c_v, d_uv) = jnp.split(h @ w_in[i], offsets, axis=-1)

        q = axial_rope(rms_norm(a_q.reshape(b, s, A_Q_HEADS, A_HEAD_DIM), g_qnorm[i]), row, col)
        k = axial_rope(rms_norm(a_k.reshape(b, s, A_KV_HEADS, A_HEAD_DIM), g_knorm[i]), row, col)
        o_a = gqa_block_attention(q, k, a_v.reshape(b, s, A_KV_HEADS, A_HEAD_DIM))

        o_b = bidir_ssd(s_z, s_x, s_b, s_c, s_dtf, s_dtb, conv_w[i], conv_b[i], dt_bias_f[i], dt_bias_b[i],
                        a_log_f[i], a_log_b[i], d_skip[i], g_ssm[i])

        lam_init = 0.8 - 0.6 * math.exp(-0.3 * i)
        lam = (jnp.exp(jnp.sum(lambda_q1[i].astype(f32) * lambda_k1[i].astype(f32)))
               - jnp.exp(jnp.sum(lambda_q2[i].astype(f32) * lambda_k2[i].astype(f32))) + lam_init)
        cq = c_q.reshape(b, s, DIFF_HEADS, 2, DIFF_QK_DIM)
        ck = c_k.reshape(b, s, DIFF_HEADS, 2, DIFF_QK_DIM)
        o_c = diff_block_attention(cq[:, :, :, 0], cq[:, :, :, 1], ck[:, :, :, 0], ck[:, :, :, 1],
                                   c_v.reshape(b, s, DIFF_HEADS, DIFF_V_DIM), lam, rel_bias)
        o_c = (rms_norm(o_c, g_diff[i]) * (1.0 - lam_init)).reshape(b, s, DIFF_HEADS * DIFF_V_DIM)

        o_d = spatial_gating(d_uv, g_sgu[i], w_spatial[i], b_spatial[i])

        branches = jnp.stack([o_a, o_b, o_c, o_d], axis=2)
        gates = jax.nn.sigmoid(h @ w_branch_gate[i]).reshape(b, s, N_BRANCHES, D_MODEL)
        merged = jnp.sum(gates * jnp.einsum('bsnc,ncd->bsnd', branches, w_branch[i]), axis=2)
        x = x + merged @ w_out[i]

        x = x + expert_choice_moe(rms_norm(x, g_moe[i]), w_router[i], w_exp_gate[i], w_exp_up[i], w_exp_down[i])

        ple_gate = jax.nn.sigmoid(rms_norm(x, g_ple[i]) @ w_ple_gate[i])
        x = x + ple_gate * (p[i] @ w_ple[i])

    return rms_norm(x, g_final)
```

```python
import math
from contextlib import ExitStack

import numpy as np
import ml_dtypes
import concourse.bass as bass
import concourse.mybir as mybir
from concourse.bass_utils import run_bass_kernel_spmd

F32 = mybir.dt.float32
BF16 = mybir.dt.bfloat16
I32 = mybir.dt.int32
AF = mybir.ActivationFunctionType
ALU = mybir.AluOpType
AX = mybir.AxisListType
NPBF = ml_dtypes.bfloat16

D = 1024
B = 2
S = 16384
DEPTH = 4
NCORE = 8
TQ = S // 4
EPS = 1e-6
CAP = 2048


class Buf:
    __slots__ = ("t", "name", "w", "r", "war", "dsem", "is_dram", "is_psum")

    def __init__(self, t, name, is_dram=False):
        self.is_dram = is_dram
        self.is_psum = False
        self.t = t
        self.name = name
        self.w = {}
        self.r = {}
        self.war = {}
        self.dsem = None

    def __getitem__(self, idx):
        return self.t[idx]


class DSem:
    __slots__ = ("sem", "cnt")

    def __init__(self, sem):
        self.sem = sem
        self.cnt = 0


class KB:
    def __init__(self):
        self.nc = bass.Bass("TRN2", target_bir_lowering=False)
        self.es = ExitStack()
        nc = self.nc
        self.eng = dict(pe=nc.tensor, dve=nc.vector, act=nc.scalar, pool=nc.gpsimd, sp=nc.sync)
        self.esem = {}
        self.ecnt = {}
        self.known = {}
        self.sems = {}
        for e in self.eng:
            s = self.es.enter_context(nc.semaphore("es_" + e))
            self.esem[e] = s
            self.ecnt[e] = 0
            self.known[e] = {}
            self.sems[id(s)] = s
        self.dsems = []
        self.nbuf = 0
        self.out_tokens = []

    def sb(self, name, shape, dt):
        self.nbuf += 1
        t = self.es.enter_context(self.nc.sbuf_tensor(f"{name}_{self.nbuf}", list(shape), dt))
        return Buf(t, name)

    def ps(self, name, shape, dt):
        self.nbuf += 1
        t = self.es.enter_context(self.nc.psum_tensor(f"{name}_{self.nbuf}", list(shape), dt))
        b = Buf(t, name)
        b.is_psum = True
        return b

    def dram(self, name, shape, dt, kind):
        t = self.nc.dram_tensor(name, list(shape), dt, kind=kind)
        b = Buf(t.ap(), name, True)
        return b

    def new_dsem(self, name):
        self.nbuf += 1
        s = self.es.enter_context(self.nc.semaphore(f"ds_{name}_{self.nbuf}"))
        d = DSem(s)
        self.sems[id(s)] = s
        self.dsems.append(d)
        return d

    def _wait(self, e, need):
        kn = self.known[e]
        engine = self.eng[e]
        for sid, val in need.items():
            if e == "pe" and sid == id(self.esem["pe"]):
                continue
            if kn.get(sid, 0) < val:
                engine.wait_ge(self.sems[sid], val)
                kn[sid] = val

    @staticmethod
    def _merge(dst, src):
        for k, v in src.items():
            if dst.get(k, 0) < v:
                dst[k] = v

    def _hazards(self, reads, writes):
        need = {}
        for b in reads:
            self._merge(need, b.w)
            if b.is_psum:
                self._merge(need, b.r)
        for w in writes:
            part = False
            if isinstance(w, tuple):
                w, part = w[0], True
            if w.r:
                w.war = dict(w.r)
                w.r = {}
                if part:
                    w.w = {}
            self._merge(need, w.war)
            if not part:
                self._merge(need, w.w)
        return need

    def _record(self, reads, writes, sid, val):
        for b in reads:
            if b.r.get(sid, 0) < val:
                b.r[sid] = val
        for w in writes:
            if isinstance(w, tuple):
                w = w[0]
                if w.w.get(sid, 0) < val:
                    w.w[sid] = val
            else:
                w.w = {sid: val}

    def op(self, e, fn, reads=(), writes=()):
        need = self._hazards(reads, writes)
        self._wait(e, need)
        ins = fn(self.eng[e])
        self.ecnt[e] += 1
        ins.then_inc(self.esem[e], 1)
        self._record(reads, writes, id(self.esem[e]), self.ecnt[e])
        return ins

    def dma(self, q, out_ap, in_ap, reads=(), writes=(), dsem=None, is_output=False, **kw):
        need = self._hazards(reads, writes)
        self._wait(q, need)
        if dsem is None:
            for b in list(writes) + list(reads):
                bb = b[0] if isinstance(b, tuple) else b
                if bb.is_dram:
                    continue
                if bb.dsem is None:
                    bb.dsem = self.new_dsem(bb.name)
                dsem = bb.dsem
                break
        assert dsem is not None
        ins = self.eng[q].dma_start(out=out_ap, in_=in_ap, **kw)
        dsem.cnt += 16
        ins.then_inc(dsem.sem, 16)
        self._record(reads, writes, id(dsem.sem), dsem.cnt)
        return ins

    def idma(self, fn, reads=(), writes=(), dsem=None):
        need = self._hazards(reads, writes)
        self._wait("pool", need)
        ins = fn(self.eng["pool"])
        dsem.cnt += 16
        ins.then_inc(dsem.sem, 16)
        self._record(reads, writes, id(dsem.sem), dsem.cnt)
        return ins

    def finish(self):
        need = {}
        for e in self.eng:
            if e != "sp":
                need[id(self.esem[e])] = self.ecnt[e]
        for d in self.dsems:
            need[id(d.sem)] = d.cnt
        need = {k: v for k, v in need.items() if v > 0}
        self._wait("sp", need)
        self.es.close()
        return self.nc


def _bc(ap_1d_dram, n, parts=128):
    return ap_1d_dram.partition_broadcast(parts) if hasattr(ap_1d_dram, "partition_broadcast") else ap_1d_dram


_WIN_SEGS = [
    (0, 256, 0),
    (256, 384, 256),
    (768, 1536, 384),
    (1544, 1800, 1152),
    (1800, 2056, 1408),
    (1536, 1544, 1664),
    (384, 512, 1672),
    (512, 768, 1800),
    (2056, 2312, 2056),
    (2312, 2824, 2312),
]
C_QA, C_KA, C_XBC, C_CQ, C_CK, C_DT, C_VA, C_Z, C_CV, C_UV = 0, 256, 384, 1152, 1408, 1664, 1672, 1800, 2056, 2312


def load_cast_weight(k, w_dram, kchunks, ncols, wbf, g_sb, stage_bufs, segs=None, qs=("sp", "act"), cast=("pool", "dve")):
    if segs is None:
        segs = [(0, ncols, 0)]
    i = 0
    for kc in range(kchunks):
        for (s0, s1, d0) in segs:
            n = s1 - s0
            for c0 in range(0, n, 1024):
                c1 = min(n, c0 + 1024)
                st = stage_bufs[i % len(stage_bufs)]
                q = qs[i % len(qs)]
                k.dma(q, st[:, 0:c1 - c0], w_dram[kc * 128:(kc + 1) * 128, s0 + c0:s0 + c1], reads=[w_dram], writes=[st])
                e = cast[i % len(cast)]
                dst = wbf[:, kc, d0 + c0:d0 + c1]
                src = st[:, 0:c1 - c0]
                if g_sb is not None:
                    k.op(e, lambda en, dst=dst, src=src, kc=kc: en.tensor_scalar(out=dst, in0=src, scalar1=g_sb[:, kc:kc + 1], scalar2=None, op0=ALU.mult),
                         reads=[st, g_sb], writes=[(wbf,)])
                else:
                    k.op(e, lambda en, dst=dst, src=src: en.tensor_copy(out=dst, in_=src), reads=[st], writes=[(wbf,)])
                i += 1


PA_SKIP = set()


def build_PA(prefix=False, final=False, TQ=TQ):
    k = KB()
    nc = k.nc
    NB = TQ // 512
    xin = k.dram("x", [TQ, D], F32, "ExternalInput")
    if prefix:
        parts = k.dram("parts", [4, TQ, D], F32, "ExternalInput")
        pin = k.dram("p", [TQ, 256], F32, "ExternalInput")
        wpg = k.dram("w_ple_gate", [D, D], F32, "ExternalInput")
        wpl = k.dram("w_ple", [256, D], F32, "ExternalInput")
        gple = k.dram("g_ple", [128, 8], F32, "ExternalInput")
    if final:
        gfin = k.dram("g_final", [D], F32, "ExternalInput")
        yout = k.dram("y", [TQ, D], F32, "ExternalOutput")
    else:
        win = k.dram("w_in", [D, 2824], F32, "ExternalInput")
        gmix = k.dram("g_mix", [128, 8], F32, "ExternalInput")
        gqk = k.dram("gqk", [128, 4], F32, "ExternalInput")
        ctd = k.dram("rope_c", [128, TQ], F32, "ExternalInput")
        std = k.dram("rope_s", [128, TQ], F32, "ExternalInput")
        permd = k.dram("perm", [128, 128], BF16, "ExternalInput")
        onesd = k.dram("onesblk", [128, 128], F32, "ExternalInput")
        gsgud = k.dram("g_sgu", [256], F32, "ExternalInput")
        wsTd = k.dram("wsT", [128, 4, 128], F32, "ExternalInput")
        bspd = k.dram("bsp", [128, 4], F32, "ExternalInput")
        o_qAT = k.dram("qAT", [256, TQ], BF16, "ExternalOutput")
        o_kAT = k.dram("kAT", [128, TQ], BF16, "ExternalOutput")
        o_vA = k.dram("vA", [TQ, 128], BF16, "ExternalOutput")
        o_qCT = k.dram("qCT", [256, TQ], BF16, "ExternalOutput")
        o_kCT = k.dram("kCT", [256, TQ], BF16, "ExternalOutput")
        o_vC = k.dram("vC", [TQ, 256], BF16, "ExternalOutput")
        o_z = k.dram("z", [TQ, 256], F32, "ExternalOutput")
        o_xbcT = k.dram("xbcT", [768, TQ], BF16, "ExternalOutput")
        o_dtT = k.dram("dtT", [8, TQ], F32, "ExternalOutput")
        o_od = k.dram("od", [TQ, 256], BF16, "ExternalOutput")
    if prefix and not final:
        o_x = k.dram("xo", [TQ, D], F32, "ExternalOutput")
    identd = k.dram("ident", [128, 128], BF16, "ExternalInput")

    ident = k.sb("ident", [128, 128], BF16)
    k.dma("sp", ident[:], identd[:, :], reads=[identd], writes=[ident])
    stage = [k.sb("wstage", [128, 1024], F32) for _ in range(3)]
    eps_t = k.sb("eps", [128, 1], F32)
    k.op("dve", lambda en: en.memset(eps_t[:], EPS), writes=[eps_t])

    xt = [k.sb("xt", [128, 4, D], F32) for _ in range(2)]
    hb = k.sb("hb", [128, D], BF16)
    hT = [k.sb("hT", [128, 8, 512], BF16) for _ in range(2)]
    junk = k.sb("junk", [128, D], BF16)
    ss = k.sb("ss", [128, 8], F32)
    pT = [k.ps("pT", [128, D], BF16) for _ in range(2)]
    pb = [k.ps("pb", [128, 512], F32) for _ in range(6)]
    pbi = [0]

    def bank():
        b = pb[pbi[0] % len(pb)]
        pbi[0] += 1
        return b

    def rms_to_hT(xsrc_ap, xbuf, hT_b, j, g_unused=None):
        k.op("act", lambda en: en.activation(out=junk[:], in_=xsrc_ap, func=AF.Square, accum_out=ss[:, 0:1]),
             reads=[xbuf], writes=[junk, ss])
        k.op("act", lambda en: en.activation(out=ss[:, 1:2], in_=ss[:, 0:1], func=AF.Sqrt, bias=eps_t[:, 0:1], scale=1.0 / D),
             reads=[ss, eps_t], writes=[ss])
        k.op("dve", lambda en: en.reciprocal(out=ss[:, 2:3], in_=ss[:, 1:2]), reads=[ss], writes=[ss])
        k.op("dve", lambda en: en.tensor_scalar(out=hb[:], in0=xsrc_ap, scalar1=ss[:, 2:3], scalar2=None, op0=ALU.mult),
             reads=[xbuf, ss], writes=[hb])
        pt = pT[j % 2]
        for kc in range(8):
            k.op("pe", lambda en, kc=kc: en.transpose(out=pt[:, kc * 128:(kc + 1) * 128], in_=hb[:, kc * 128:(kc + 1) * 128], identity=ident[:]),
                 reads=[hb, ident], writes=[(pt,)])
        k.op("act", lambda en: en.activation(out=hT_b[:, :, j * 128:(j + 1) * 128], in_=pt[:].rearrange("p (c t) -> p c t", c=8), func=AF.Copy),
             reads=[pt], writes=[(hT_b,)])

    if not final:
        wbf = k.sb("wbf", [128, 8, 2824], BF16)
        gmix_sb = k.sb("gmix", [128, 8], F32)
        k.dma("sp", gmix_sb[:], gmix[:, :], reads=[gmix], writes=[gmix_sb])
        load_cast_weight(k, win, 8, 2824, wbf, gmix_sb, stage, segs=_WIN_SEGS)
        gqk_sb = k.sb("gqk", [128, 4], F32)
        k.dma("sp", gqk_sb[:], gqk[:, :], reads=[gqk], writes=[gqk_sb])
        perm = k.sb("perm", [128, 128], BF16)
        k.dma("sp", perm[:], permd[:, :], reads=[permd], writes=[perm])
        onesb = k.sb("onesb", [128, 128], F32)
        k.dma("sp", onesb[:], onesd[:, :], reads=[onesd], writes=[onesb])
        gsgu = k.sb("gsgu", [128, 256], F32)
        k.dma("sp", gsgu[:], gsgud[:].partition_broadcast(128), reads=[gsgud], writes=[gsgu])
        wsf = k.sb("wsf", [128, 4, 128], F32)
        k.dma("sp", wsf[:], wsTd[:, :, :], reads=[wsTd], writes=[wsf])
        wsb = k.sb("wsb", [128, 4, 128], BF16)
        k.op("dve", lambda en: en.tensor_copy(out=wsb[:], in_=wsf[:]), reads=[wsf], writes=[wsb])
        bsp = k.sb("bsp", [128, 4], F32)
        k.dma("sp", bsp[:], bspd[:, :], reads=[bspd], writes=[bsp])
        ct = [k.sb("ct", [128, 512], F32) for _ in range(2)]
        stb = [k.sb("st", [128, 512], F32) for _ in range(2)]
        sg_fm = [k.sb("sg_fm", [128, 512], BF16) for _ in range(4)]
        sg_dt = [k.sb("sg_dt", [8, 512], F32) for _ in range(2)]
        sg_vA = [k.sb("sg_vA", [128, 128], BF16) for _ in range(2)]
        sg_z = [k.sb("sg_z", [128, 256], F32) for _ in range(2)]
        sg_vC = [k.sb("sg_vC", [128, 256], BF16) for _ in range(2)]
        sg_od = [k.sb("sg_od", [128, 256], BF16) for _ in range(2)]
        sq = k.sb("sq", [128, 512], F32)
        rstd = k.sb("rstd", [128, 512], F32)
        qg = k.sb("qg", [128, 512], BF16)
        t1 = k.sb("t1", [128, 512], F32)
        t2 = k.sb("t2", [128, 512], F32)
        gel = k.sb("gel", [128, 512], F32)
        vn = k.sb("vn", [128, 256], BF16)
        fmi = [0]

        for blk in range(NB):
            xb = xt[blk % 2]
            hTb = hT[blk % 2]
            t0 = blk * 512
            k.dma("sp", xb[:], xin[t0:t0 + 512, :].rearrange("(j p) d -> p j d", p=128), reads=[xin], writes=[xb])
            k.dma("act", ct[blk % 2][:], ctd[:, t0:t0 + 512], reads=[ctd], writes=[ct[blk % 2]])
            k.dma("act", stb[blk % 2][:], std[:, t0:t0 + 512], reads=[std], writes=[stb[blk % 2]])
            for j in range(4):
                rms_to_hT(xb[:, j, :], xb, hTb, j)

            def fm_chunk(col0, m=128):
                ps = bank()
                for kc in range(8):
                    k.op("pe", lambda en, kc=kc: en.matmul(ps[0:m, :], lhsT=wbf[:, kc, col0:col0 + m], rhs=hTb[:, kc, :], start=(kc == 0), stop=(kc == 7)),
                         reads=[wbf, hTb], writes=[(ps,)])
                return ps

            def fm_out(src_ps, dram, row0):
                sg = sg_fm[fmi[0] % 4]
                fmi[0] += 1
                e = ("act", "dve")[fmi[0] % 2]
                if e == "act":
                    k.op("act", lambda en: en.activation(out=sg[:], in_=src_ps[:], func=AF.Copy), reads=[src_ps], writes=[sg])
                else:
                    k.op("dve", lambda en: en.tensor_copy(out=sg[:], in_=src_ps[:]), reads=[src_ps], writes=[sg])
                k.dma("sp", dram[row0:row0 + 128, t0:t0 + 512], sg[:], reads=[sg], writes=[(dram,)])

            for (col0, dram, row0, gi) in ((C_QA, o_qAT, 0, 0), (C_QA + 128, o_qAT, 128, 0), (C_KA, o_kAT, 0, 2)) if 'qk' not in PA_SKIP else ():
                ps = fm_chunk(col0)
                k.op("act", lambda en: en.activation(out=sq[:], in_=ps[:], func=AF.Square), reads=[ps], writes=[sq])
                ps2 = bank()
                k.op("pe", lambda en: en.matmul(ps2[:], lhsT=onesb[:], rhs=sq[:], start=True, stop=True), reads=[onesb, sq], writes=[ps2])
                k.op("act", lambda en: en.activation(out=rstd[:], in_=ps2[:], func=AF.Sqrt, bias=eps_t[:, 0:1], scale=1.0), reads=[ps2, eps_t], writes=[rstd])
                k.op("dve", lambda en: en.reciprocal(out=rstd[:], in_=rstd[:]), reads=[rstd], writes=[rstd])
                k.op("dve", lambda en: en.scalar_tensor_tensor(out=qg[:], in0=ps[:], scalar=gqk_sb[:, gi:gi + 1], in1=rstd[:], op0=ALU.mult, op1=ALU.mult),
                     reads=[ps, gqk_sb, rstd], writes=[qg])
                ps3 = bank()
                k.op("pe", lambda en: en.matmul(ps3[:], lhsT=perm[:], rhs=qg[:], start=True, stop=True), reads=[perm, qg], writes=[ps3])
                k.op("dve", lambda en: en.tensor_tensor(out=t1[:], in0=qg[:], in1=ct[blk % 2][:], op=ALU.mult), reads=[qg, ct[blk % 2]], writes=[t1])
                k.op("dve", lambda en: en.tensor_tensor(out=t2[:], in0=ps3[:], in1=stb[blk % 2][:], op=ALU.mult), reads=[ps3, stb[blk % 2]], writes=[t2])
                sg = sg_fm[fmi[0] % 4]
                fmi[0] += 1
                k.op("dve", lambda en: en.tensor_tensor(out=sg[:], in0=t1[:], in1=t2[:], op=ALU.add), reads=[t1, t2], writes=[sg])
                k.dma("sp", dram[row0:row0 + 128, t0:t0 + 512], sg[:], reads=[sg], writes=[(dram,)])
            for c in range(6 if 'fm' not in PA_SKIP else 0):
                fm_out(fm_chunk(C_XBC + c * 128), o_xbcT, c * 128)
            for c in range(2):
                fm_out(fm_chunk(C_CQ + c * 128), o_qCT, c * 128)
            for c in range(2):
                fm_out(fm_chunk(C_CK + c * 128), o_kCT, c * 128)
            ps = fm_chunk(C_DT, m=8)
            sd = sg_dt[blk % 2]
            k.op("act", lambda en: en.activation(out=sd[:], in_=ps[0:8, :], func=AF.Copy), reads=[ps], writes=[sd])
            k.dma("sp", o_dtT[:, t0:t0 + 512], sd[:], reads=[sd], writes=[(o_dtT,)])

            for j in range(4 if 'tm' not in PA_SKIP else 0):
                r0 = t0 + j * 128

                def tm_group(col0, n):
                    ps = bank()
                    for kc in range(8):
                        k.op("pe", lambda en, kc=kc: en.matmul(ps[:, 0:n], lhsT=hTb[:, kc, j * 128:(j + 1) * 128], rhs=wbf[:, kc, col0:col0 + n], start=(kc == 0), stop=(kc == 7)),
                             reads=[wbf, hTb], writes=[(ps,)])
                    return ps
                if 'va' not in PA_SKIP:
                    ps = tm_group(C_VA, 128)
                    sv = sg_vA[j % 2]
                    k.op("act", lambda en: en.activation(out=sv[:], in_=ps[:, 0:128], func=AF.Copy), reads=[ps], writes=[sv])
                    k.dma("sp", o_vA[r0:r0 + 128, :], sv[:], reads=[sv], writes=[(o_vA,)])
                if 'zc' not in PA_SKIP:
                    ps = tm_group(C_Z, 512)
                    sz = sg_z[j % 2]
                    k.op("act", lambda en: en.activation(out=sz[:], in_=ps[:, 0:256], func=AF.Copy), reads=[ps], writes=[sz])
                    k.dma("sp", o_z[r0:r0 + 128, :], sz[:], reads=[sz], writes=[(o_z,)])
                    sc = sg_vC[j % 2]
                    k.op("act", lambda en: en.activation(out=sc[:], in_=ps[:, 256:512], func=AF.Copy), reads=[ps], writes=[sc])
                    k.dma("sp", o_vC[r0:r0 + 128, :], sc[:], reads=[sc], writes=[(o_vC,)])
                if 'sgu' in PA_SKIP:
                    continue
                ps = tm_group(C_UV, 512)
                k.op("act", lambda en: en.activation(out=gel[:], in_=ps[:], func=AF.Gelu_apprx_tanh), reads=[ps], writes=[gel])
                k.op("act", lambda en: en.activation(out=junk[:, 0:256], in_=gel[:, 256:512], func=AF.Square, accum_out=ss[:, 4:5]), reads=[gel], writes=[junk, ss])
                k.op("act", lambda en: en.activation(out=ss[:, 5:6], in_=ss[:, 4:5], func=AF.Sqrt, bias=eps_t[:, 0:1], scale=1.0 / 256), reads=[ss, eps_t], writes=[ss])
                k.op("dve", lambda en: en.reciprocal(out=ss[:, 6:7], in_=ss[:, 5:6]), reads=[ss], writes=[ss])
                k.op("dve", lambda en: en.scalar_tensor_tensor(out=vn[:], in0=gel[:, 256:512], scalar=ss[:, 6:7], in1=gsgu[:], op0=ALU.mult, op1=ALU.mult),
                     reads=[gel, ss, gsgu], writes=[vn])
                ps2 = bank()
                for g in range(4):
                    k.op("pe", lambda en, g=g: en.matmul(ps2[:, g * 64:(g + 1) * 64], lhsT=wsb[:, g, :], rhs=vn[:, g * 64:(g + 1) * 64], start=True, stop=True),
                         reads=[wsb, vn], writes=[(ps2,)])
                so = sg_od[j % 2]
                for g in range(4):
                    k.op("dve", lambda en, g=g: en.scalar_tensor_tensor(out=so[:, g * 64:(g + 1) * 64], in0=ps2[:, g * 64:(g + 1) * 64], scalar=bsp[:, g:g + 1],
                                                                       in1=gel[:, g * 64:(g + 1) * 64], op0=ALU.add, op1=ALU.mult),
                         reads=[ps2, bsp, gel], writes=[(so,)])
                k.dma("sp", o_od[r0:r0 + 128, :], so[:], reads=[so], writes=[(o_od,)])
    return k.finish()


def _rope_tables():
    t = np.arange(S)
    row = (t // 64).astype(np.float32)
    col = (t % 64).astype(np.float32)
    freqs = (10000.0 ** (-np.arange(0, 32, 2, dtype=np.float32) / 32)).astype(np.float32)
    ct = np.zeros((128, S), np.float32)
    st = np.zeros((128, S), np.float32)
    perm = np.zeros((128, 128), np.float32)
    for p in range(128):
        d = p % 64
        half = d // 32
        w = d % 32
        m = w % 16
        pos = row if half == 0 else col
        ang = pos * freqs[m]
        ct[p] = np.cos(ang)
        st[p] = -np.sin(ang) if w < 16 else np.sin(ang)
        partner = p + 16 if w < 16 else p - 16
        perm[partner, p] = 1.0
    return ct, st, perm


def _partner_perm_vec(g64):
    g = np.concatenate([g64, g64]).astype(np.float32)
    out = np.zeros(128, np.float32)
    for p in range(128):
        w = (p % 64) % 32
        partner = p + 16 if w < 16 else p - 16
        out[p] = g[partner]
    return g, out


_CONST = {}


def consts():
    if not _CONST:
        ct, st, perm = _rope_tables()
        _CONST["ct"], _CONST["st"] = ct, st
        _CONST["perm"] = perm.astype(NPBF)
        _CONST["ident"] = np.eye(128, dtype=np.float32).astype(NPBF)
        ob = np.zeros((128, 128), np.float32)
        ob[:64, :64] = 1.0 / 64
        ob[64:, 64:] = 1.0 / 64
        _CONST["onesblk"] = ob
    return _CONST


_PROG = {}


def prog(name, fn, *a):
    key = (name,) + a
    if key not in _PROG:
        _PROG[key] = fn(*a)
    return _PROG[key]


def run(nc, in_maps):
    res = run_bass_kernel_spmd(nc, in_maps, core_ids=list(range(NCORE)))
    return res.results


def pa_inputs(i, inp, c, xs):
    C = consts()
    b, r = c // 4, c % 4
    t0 = r * TQ
    gq, gqr = _partner_perm_vec(inp["g_qnorm"][i])
    gk, gkr = _partner_perm_vec(inp["g_knorm"][i])
    return {
        "x": xs[c],
        "w_in": inp["w_in"][i],
        "g_mix": np.ascontiguousarray(inp["g_mix"][i].reshape(8, 128).T),
        "gqk": np.ascontiguousarray(np.stack([gq, gqr, gk, gkr], axis=1)),
        "rope_c": np.ascontiguousarray(C["ct"][:, t0:t0 + TQ]),
        "rope_s": np.ascontiguousarray(C["st"][:, t0:t0 + TQ]),
        "perm": C["perm"], "onesblk": C["onesblk"], "ident": C["ident"],
        "g_sgu": inp["g_sgu"][i],
        "wsT": np.ascontiguousarray(inp["w_spatial"][i].transpose(2, 0, 1)),
        "bsp": np.ascontiguousarray(inp["b_spatial"][i].T),
    }


def _t5_bucket(rel):
    nb, max_exact = 16, 8
    ret = np.where(rel > 0, nb, 0)
    r = np.abs(rel)
    rf = np.maximum(r, 1).astype(np.float32)
    large = max_exact + (np.log(rf / max_exact) / np.float32(math.log(128 / max_exact)) * (nb - max_exact)).astype(np.int32)
    large = np.minimum(large, nb - 1)
    return ret + np.where(r < max_exact, r, large)


def _bias_bucket_strip():
    out = np.zeros((128, 9 * 128), np.float32)
    kk = np.arange(128)[:, None]
    qq = np.arange(128)[None, :]
    for c in range(9):
        dlt = 4 - c
        out[:, c * 128:(c + 1) * 128] = _t5_bucket(128 * dlt + kk - qq).astype(np.float32)
    return out


def build_PM_attn(mode, NQ=TQ, NK=S):
    k = KB()
    NKT = NK // 128
    NQB = NQ // 512
    if mode == "A":
        qA = k.dram("qA", [128, 2, NQ], BF16, "ExternalInput")
        kA = k.dram("kA", [128, NK], BF16, "ExternalInput")
        vA = k.dram("vA", [NK, 2, 65], BF16, "ExternalInput")
    else:
        qC = k.dram("qC", [128, 2, 2, NQ], BF16, "ExternalInput")
        kC = k.dram("kC", [128, 2, NK], BF16, "ExternalInput")
        vC = k.dram("vC", [NK, 4, 65], BF16, "ExternalInput")
    bidxd = k.dram("bidx", [128, 9 * 128], F32, "ExternalInput")
    relbd = k.dram("relb", [128], F32, "ExternalInput")
    lamvd = k.dram("lamv", [128], F32, "ExternalInput")
    lamid = k.dram("lami", [2], F32, "ExternalInput")
    gdifd = k.dram("gdiff", [64], F32, "ExternalInput")
    oa = k.dram("o", [NQ, 256], F32, "ExternalOutput")

    relb = k.sb("relb", [128, 128], F32)
    k.dma("sp", relb[:], relbd[:].partition_broadcast(128), reads=[relbd], writes=[relb])
    lamv = k.sb("lamv", [128, 128], F32)
    k.dma("sp", lamv[:], lamvd[:].partition_broadcast(128), reads=[lamvd], writes=[lamv])
    lami = k.sb("lami", [128, 2], F32)
    k.dma("sp", lami[:], lamid[:].partition_broadcast(128), reads=[lamid], writes=[lami])
    gdif = k.sb("gdif", [128, 64], F32)
    k.dma("sp", gdif[:], gdifd[:].partition_broadcast(128), reads=[gdifd], writes=[gdif])
    eps_t = k.sb("eps", [128, 1], F32)
    k.op("dve", lambda en: en.memset(eps_t[:], EPS), writes=[eps_t])
    if mode == "C":
        tbd = k.dram("tb", [4 * NKT], F32, "ExternalInput")
        tb = k.sb("tb", [128, 4 * NKT], F32)
        k.dma("sp", tb[:], tbd[:].partition_broadcast(128), reads=[tbd], writes=[tb])
    sm = k.sb("sm", [128, 16], F32)
    tmp = k.sb("tmp", [128, 64], F32)
    k.op("dve", lambda en: en.tensor_tensor(out=tmp[:, 0:32], in0=lamv[:, 0:32], in1=lamv[:, 32:64], op=ALU.mult), reads=[lamv], writes=[tmp])
    k.op("dve", lambda en: en.tensor_tensor(out=tmp[:, 32:64], in0=lamv[:, 64:96], in1=lamv[:, 96:128], op=ALU.mult), reads=[lamv], writes=[tmp])
    k.op("dve", lambda en: en.reduce_sum(out=sm[:, 0:1], in_=tmp[:, 0:32], axis=AX.X), reads=[tmp], writes=[sm])
    k.op("dve", lambda en: en.reduce_sum(out=sm[:, 1:2], in_=tmp[:, 32:64], axis=AX.X), reads=[tmp], writes=[sm])
    k.op("act", lambda en: en.activation(out=sm[:, 2:4], in_=sm[:, 0:2], func=AF.Exp), reads=[sm], writes=[sm])
    k.op("dve", lambda en: en.tensor_tensor(out=sm[:, 4:5], in0=sm[:, 2:3], in1=sm[:, 3:4], op=ALU.subtract), reads=[sm], writes=[sm])
    k.op("dve", lambda en: en.tensor_tensor(out=sm[:, 5:6], in0=sm[:, 4:5], in1=lami[:, 0:1], op=ALU.add), reads=[sm, lami], writes=[sm])
    k.op("dve", lambda en: en.tensor_scalar(out=gdif[:], in0=gdif[:], scalar1=lami[:, 1:2], scalar2=None, op0=ALU.mult), reads=[gdif, lami], writes=[gdif])
    bidx = k.sb("bidx", [128, 1152], F32)
    k.dma("sp", bidx[:], bidxd[:, :], reads=[bidxd], writes=[bidx])
    LS = [k.sb("LS", [128, 1152], F32) for _ in range(4)]
    oh = k.sb("oh", [128, 1152], F32)
    for h in range(4):
        k.op("pool", lambda en, h=h: en.memset(LS[h][:], 0.0), writes=[LS[h]])
    for bkt in range(32 if mode == "C" else 0):
        k.op("dve", lambda en, bkt=bkt: en.tensor_scalar(out=oh[:], in0=bidx[:], scalar1=float(bkt), scalar2=None, op0=ALU.is_equal), reads=[bidx], writes=[oh])
        for h in range(4):
            k.op("dve", lambda en, h=h, bkt=bkt: en.scalar_tensor_tensor(out=LS[h][:], in0=oh[:], scalar=relb[:, bkt * 4 + h:bkt * 4 + h + 1], in1=LS[h][:], op0=ALU.mult, op1=ALU.add),
                 reads=[oh, relb, LS[h]], writes=[LS[h]])

    pS = [k.ps("pS", [128, 512], F32) for _ in range(4)]
    pO = [k.ps("pO", [128, 4, 128], F32) for _ in range(4)]
    pT = [k.sb("pT", [128, 512], BF16) for _ in range(4)]
    lg = [k.sb("lg", [128, 512], F32) for _ in range(2)]
    ost = [k.sb("ost", [128, 4, 256], F32)] * 2
    cnt = dict(s=0, t=0, o=0, l=0)

    def unit(KTb, kap_fn, qap, Vb, vslot, po, qb, bias_h=None):
        LA = 2
        pss = {}

        def qk(kt_):
            ps_ = pS[cnt["s"] % 4]
            cnt["s"] += 1
            k.op("pe", lambda en: en.matmul(ps_[:], lhsT=kap_fn(kt_), rhs=qap, start=True, stop=True), reads=[KTb[0], KTb[1]], writes=[ps_])
            pss[kt_] = ps_
        for kt in range(min(LA, NKT)):
            qk(kt)
        for kt in range(NKT):
            if kt + LA < NKT:
                qk(kt + LA)
            ps = pss.pop(kt)
            pt = pT[cnt["t"] % 4]
            cnt["t"] += 1
            if bias_h is None:
                k.op("act", lambda en: en.activation(out=pt[:], in_=ps[:], func=AF.Exp, scale=0.125), reads=[ps], writes=[pt])
            else:
                d0 = (kt - 1) - 4 * qb
                sc = 32 ** -0.5
                if kt > NQ // 128 + 1:
                    bcol = bias_h * NKT + kt
                    k.op("act", lambda en: en.activation(out=pt[:], in_=ps[:], func=AF.Exp, scale=sc, bias=tb[:, bcol:bcol + 1]), reads=[ps, tb], writes=[pt])
                elif d0 >= 5 or d0 <= -2:
                    bcol = (31 if d0 >= 5 else 15) * 4 + bias_h
                    k.op("act", lambda en: en.activation(out=pt[:], in_=ps[:], func=AF.Exp, scale=sc, bias=relb[:, bcol:bcol + 1]), reads=[ps, relb], writes=[pt])
                else:
                    l = lg[cnt["l"] % 2]
                    cnt["l"] += 1
                    c0 = (4 - d0) * 128
                    k.op("dve", lambda en: en.scalar_tensor_tensor(out=l[:], in0=ps[:], scalar=sc, in1=LS[bias_h][:, c0:c0 + 512], op0=ALU.mult, op1=ALU.add),
                         reads=[ps, LS[bias_h]], writes=[l])
                    k.op("act", lambda en: en.activation(out=pt[:], in_=l[:], func=AF.Exp), reads=[l], writes=[pt])
            for j in range(4):
                k.op("pe", lambda en, j=j: en.matmul(po[:, j, 0:65], lhsT=pt[:, j * 128:(j + 1) * 128], rhs=Vb[:, kt, vslot, :], start=(kt == 0 and j == 0), stop=(kt == NKT - 1 and j == 3), skip_group_check=True),
                     reads=[pt, Vb], writes=[(po,)])

    if mode == "A":
        KT = k.sb("KT", [128, NK], BF16)
        QT = k.sb("QT", [128, 2, NQ], BF16)
        V = k.sb("V", [128, NKT, 2, 65], BF16)
        for i4 in range(4):
            n0, n1 = (NKT * i4 // 4) * 128, (NKT * (i4 + 1) // 4) * 128
            k.dma(("sp", "act")[i4 % 2], KT[:, n0:n1], kA[:, n0:n1], reads=[kA], writes=[(KT,)])
            k.dma(("act", "sp")[i4 % 2], V[:, n0 // 128:n1 // 128], vA[n0:n1].rearrange("(t p) g d -> p t g d", p=128), reads=[vA], writes=[(V,)])
        k.dma("sp", QT[:], qA[:, :, :], reads=[qA], writes=[QT])
        for qb in range(NQB):
            os_ = ost[qb % 2]
            for h in range(4):
                g, r = h // 2, h % 2
                po = pO[cnt["o"] % 4]
                cnt["o"] += 1
                unit((KT, QT), lambda kt, g=g: KT[g * 64:(g + 1) * 64, kt * 128:(kt + 1) * 128], QT[g * 64:(g + 1) * 64, r, qb * 512:(qb + 1) * 512], V, g, po, qb)
                k.op("dve", lambda en: en.reciprocal(out=sm[:, 8:12], in_=po[:, :, 64]), reads=[po], writes=[sm])
                for j in range(4):
                    k.op("dve", lambda en, j=j: en.tensor_scalar(out=os_[:, j, h * 64:(h + 1) * 64], in0=po[:, j, 0:64], scalar1=sm[:, 8 + j:9 + j], scalar2=None, op0=ALU.mult),
                         reads=[po, sm], writes=[(os_,)])
            k.dma("sp", oa[qb * 512:(qb + 1) * 512, :].rearrange("(j p) c -> p j c", p=128), os_[:], reads=[os_], writes=[(oa,)])
    else:
        KT = k.sb("KT", [128, 2, NK], BF16)
        QT = k.sb("QT", [128, 2, 2, NQ], BF16)
        V = k.sb("V", [128, NKT, 4, 65], BF16)
        for i4 in range(4):
            n0, n1 = (NKT * i4 // 4) * 128, (NKT * (i4 + 1) // 4) * 128
            k.dma(("sp", "act")[i4 % 2], KT[:, :, n0:n1], kC[:, :, n0:n1], reads=[kC], writes=[(KT,)])
            k.dma(("act", "sp")[i4 % 2], V[:, n0 // 128:n1 // 128], vC[n0:n1].rearrange("(t p) g d -> p t g d", p=128), reads=[vC], writes=[(V,)])
        k.dma("sp", QT[:], qC[:, :, :, :], reads=[qC], writes=[QT])
        ob = k.sb("ob", [128, 64], F32)
        junk = k.sb("junk", [128, 64], F32)
        for qb in range(NQB):
            os_ = ost[qb % 2]
            for h in range(4):
                c, gi = h // 2, h % 2
                pos = []
                for m in range(2):
                    po = pO[cnt["o"] % 4]
                    cnt["o"] += 1
                    pos.append(po)
                    unit((KT, QT), lambda kt, c=c, gi=gi: KT[gi * 64:(gi + 1) * 64, c, kt * 128:(kt + 1) * 128],
                         QT[gi * 64:(gi + 1) * 64, c, m, qb * 512:(qb + 1) * 512], V, h, po, qb, bias_h=h)
                po1, po2 = pos
                k.op("dve", lambda en: en.reciprocal(out=sm[:, 8:12], in_=po1[:, :, 64]), reads=[po1], writes=[sm])
                k.op("dve", lambda en: en.reciprocal(out=sm[:, 12:16], in_=po2[:, :, 64]), reads=[po2], writes=[sm])
                k.op("dve", lambda en: en.tensor_scalar(out=sm[:, 12:16], in0=sm[:, 12:16], scalar1=sm[:, 5:6], scalar2=None, op0=ALU.mult), reads=[sm], writes=[sm])
                for j in range(4):
                    k.op("dve", lambda en, j=j: en.tensor_scalar(out=tmp[:], in0=po2[:, j, 0:64], scalar1=sm[:, 12 + j:13 + j], scalar2=None, op0=ALU.mult), reads=[po2, sm], writes=[tmp])
                    k.op("dve", lambda en, j=j: en.scalar_tensor_tensor(out=ob[:], in0=po1[:, j, 0:64], scalar=sm[:, 8 + j:9 + j], in1=tmp[:], op0=ALU.mult, op1=ALU.subtract),
                         reads=[po1, sm, tmp], writes=[ob])
                    k.op("act", lambda en: en.activation(out=junk[:], in_=ob[:], func=AF.Square, accum_out=sm[:, 6:7]), reads=[ob], writes=[junk, sm])
                    k.op("act", lambda en: en.activation(out=sm[:, 7:8], in_=sm[:, 6:7], func=AF.Sqrt, bias=eps_t[:, 0:1], scale=1.0 / 64), reads=[sm, eps_t], writes=[sm])
                    k.op("dve", lambda en: en.reciprocal(out=sm[:, 7:8], in_=sm[:, 7:8]), reads=[sm], writes=[sm])
                    k.op("dve", lambda en, j=j: en.scalar_tensor_tensor(out=os_[:, j, h * 64:(h + 1) * 64], in0=ob[:], scalar=sm[:, 7:8], in1=gdif[:], op0=ALU.mult, op1=ALU.mult),
                         reads=[ob, sm, gdif], writes=[(os_,)])
            k.dma("sp", oa[qb * 512:(qb + 1) * 512, :].rearrange("(j p) c -> p j c", p=128), os_[:], reads=[os_], writes=[(oa,)])
    return k.finish()


def c_slot_order(nkt, own0, nown, rel_bias):
    order = [own0 - 1 if own0 >= 1 else -1]
    order += list(range(own0, own0 + nown))
    order.append(own0 + nown if own0 + nown < nkt else -1)
    rest = [t for t in range(nkt) if t < own0 - 1 or t > own0 + nown]
    order += rest
    while len(order) < nkt + 1:
        order.append(-1)
    ns = nkt + 1
    tb = np.zeros((4, ns), np.float32)
    for sl, t in enumerate(order):
        if t >= 0 and sl > nown + 1:
            tb[:, sl] = rel_bias[31] if t > own0 else rel_bias[15]
    return order, tb.reshape(-1)


def c_apply_order(kC, vC, order):
    ns = len(order)
    ko = np.zeros((128, 2, ns * 128), kC.dtype)
    vo = np.zeros((ns * 128, 4, 65), vC.dtype)
    for sl, t in enumerate(order):
        if t >= 0:
            ko[:, :, sl * 128:(sl + 1) * 128] = kC[:, :, t * 128:(t + 1) * 128]
            vo[sl * 128:(sl + 1) * 128] = vC[t * 128:(t + 1) * 128]
    return ko, vo


def attn_inputs_A(paouts, c):
    b, r = c // 4, c % 4
    grp = [paouts[b * 4 + i] for i in range(4)]
    qAT = np.asarray(paouts[c]["qAT"])
    qA = np.ascontiguousarray(qAT.reshape(2, 2, 64, TQ).transpose(0, 2, 1, 3).reshape(128, 2, TQ))
    kA = np.concatenate([np.asarray(g["kAT"]) for g in grp], axis=1)
    v = np.concatenate([np.asarray(g["vA"]) for g in grp], axis=0).reshape(S, 2, 64)
    vA = np.concatenate([v, np.ones((S, 2, 1), v.dtype)], axis=-1)
    return {"qA": qA, "kA": np.ascontiguousarray(kA), "vA": np.ascontiguousarray(vA)}


def attn_inputs_C(paouts, c, inp, i):
    b, r = c // 4, c % 4
    grp = [paouts[b * 4 + i_] for i_ in range(4)]
    qCT = np.asarray(paouts[c]["qCT"])
    q4 = qCT.reshape(2, 2, 2, 32, TQ)
    qC = np.zeros((2, 64, 2, 2, TQ), qCT.dtype)
    for m in range(2):
        qC[:, m * 32:(m + 1) * 32, :, m, :] = q4[:, :, m].transpose(1, 2, 0, 3)
    qC = qC.reshape(128, 2, 2, TQ)
    kCT = np.concatenate([np.asarray(g["kCT"]) for g in grp], axis=1)
    kC = np.ascontiguousarray(kCT.reshape(2, 128, S).transpose(1, 0, 2))
    v = np.concatenate([np.asarray(g["vC"]) for g in grp], axis=0).reshape(S, 4, 64)
    vC = np.concatenate([v, np.ones((S, 4, 1), v.dtype)], axis=-1)
    order, tb = c_slot_order(S // 128, r * (TQ // 128), TQ // 128, inp["rel_bias"])
    kCs, vCs = c_apply_order(kC, vC, order)
    lam_init = 0.8 - 0.6 * math.exp(-0.3 * i)
    lamv = np.concatenate([inp["lambda_q1"][i], inp["lambda_k1"][i], inp["lambda_q2"][i], inp["lambda_k2"][i]]).astype(np.float32)
    return {"qC": np.ascontiguousarray(qC), "kC": kCs, "vC": vCs, "tb": tb,
            "bidx": _bias_bucket_strip(), "relb": np.ascontiguousarray(inp["rel_bias"].reshape(-1)),
            "lamv": lamv, "lami": np.array([lam_init, 1.0 - lam_init], np.float32), "gdiff": inp["g_diff"][i]}


def _attn_common_dummy(inp, i):
    lam_init = 0.8 - 0.6 * math.exp(-0.3 * i)
    lamv = np.concatenate([inp["lambda_q1"][i], inp["lambda_k1"][i], inp["lambda_q2"][i], inp["lambda_k2"][i]]).astype(np.float32)
    return {"bidx": _bias_bucket_strip(), "relb": np.ascontiguousarray(inp["rel_bias"].reshape(-1)),
            "lamv": lamv, "lami": np.array([lam_init, 1.0 - lam_init], np.float32), "gdiff": inp["g_diff"][i]}


def build_final(TQ=TQ):
    k = KB()
    xin = k.dram("x", [TQ, D], F32, "ExternalInput")
    gd = k.dram("g_final", [D], F32, "ExternalInput")
    yo = k.dram("y", [TQ, D], F32, "ExternalOutput")
    g = k.sb("g", [128, D], F32)
    k.dma("sp", g[:], gd[:].partition_broadcast(128), reads=[gd], writes=[g])
    eps_t = k.sb("eps", [128, 1], F32)
    k.op("dve", lambda en: en.memset(eps_t[:], EPS), writes=[eps_t])
    xt = [k.sb("xt", [128, 4, D], F32) for _ in range(2)]
    yt = [k.sb("yt", [128, 4, D], F32) for _ in range(2)]
    junk = k.sb("junk", [128, D], BF16)
    ss = k.sb("ss", [128, 4], F32)
    for blk in range(TQ // 512):
        xb, yb = xt[blk % 2], yt[blk % 2]
        t0 = blk * 512
        k.dma("sp", xb[:], xin[t0:t0 + 512, :].rearrange("(j p) d -> p j d", p=128), reads=[xin], writes=[xb])
        for j in range(4):
            k.op("act", lambda en, j=j: en.activation(out=junk[:], in_=xb[:, j, :], func=AF.Square, accum_out=ss[:, 0:1]), reads=[xb], writes=[junk, ss])
            k.op("act", lambda en: en.activation(out=ss[:, 1:2], in_=ss[:, 0:1], func=AF.Sqrt, bias=eps_t[:, 0:1], scale=1.0 / D), reads=[ss, eps_t], writes=[ss])
            k.op("dve", lambda en: en.reciprocal(out=ss[:, 2:3], in_=ss[:, 1:2]), reads=[ss], writes=[ss])
            k.op("dve", lambda en, j=j: en.scalar_tensor_tensor(out=yb[:, j, :], in0=xb[:, j, :], scalar=ss[:, 2:3], in1=g[:], op0=ALU.mult, op1=ALU.mult),
                 reads=[xb, ss, g], writes=[(yb,)])
        k.dma("act", yo[t0:t0 + 512, :].rearrange("(j p) d -> p j d", p=128), yb[:], reads=[yb], writes=[(yo,)])
    return k.finish()


def ssd_inputs(i, inp, pa, c):
    C = consts()
    b, j = c // 4, c % 4
    g = j // 2
    grp = [pa[b * 4 + r] for r in range(4)]
    xbcT = np.concatenate([np.asarray(gp["xbcT"]) for gp in grp], axis=1)
    dtT = np.concatenate([np.asarray(gp["dtT"]) for gp in grp], axis=1)
    xbc = np.zeros((128, 3, S + 4), xbcT.dtype)
    xbc[0:64, 0, 2:-2] = xbcT[j * 64:(j + 1) * 64]
    xbc[:, 1, 2:-2] = xbcT[256 + g * 128:256 + (g + 1) * 128]
    xbc[:, 2, 2:-2] = xbcT[512 + g * 128:512 + (g + 1) * 128]
    cwf = inp["conv_w"][i]
    cbf = inp["conv_b"][i]
    cw = np.zeros((128, 3, 5), np.float32)
    cb = np.zeros((128, 3), np.float32)
    cw[0:64, 0] = cwf[:, j * 64:(j + 1) * 64].T
    cw[:, 1] = cwf[:, 256 + g * 128:256 + (g + 1) * 128].T
    cw[:, 2] = cwf[:, 512 + g * 128:512 + (g + 1) * 128].T
    cb[0:64, 0] = cbf[j * 64:(j + 1) * 64]
    cb[:, 1] = cbf[256 + g * 128:256 + (g + 1) * 128]
    cb[:, 2] = cbf[512 + g * 128:512 + (g + 1) * 128]
    dtr = np.ascontiguousarray(np.stack([dtT[j], dtT[4 + j]], axis=-1).reshape(S // 128, 128, 2).transpose(1, 0, 2))
    sc = np.zeros((128, 8), np.float32)
    sc[:, 0] = inp["dt_bias_f"][i][j]
    sc[:, 1] = inp["dt_bias_b"][i][j]
    sc[:, 2] = inp["a_log_f"][i][j]
    sc[:, 3] = inp["a_log_b"][i][j]
    sc[:, 4] = inp["d_skip"][i][j]
    return {"xbc": xbc, "cw": cw, "cb": cb, "dtr": dtr, "sc": sc, "triU": C["triU"], "triL": C["triL"],
            "identf": C["identf"], "ident": C["ident"]}


def _gl(v):
    return np.ascontiguousarray(np.asarray(v).reshape(8, 128).T)


def kernel(n_layers=DEPTH, **inp):
    inp = {k_: np.asarray(v) for k_, v in inp.items()}
    C = consts()
    if "triU" not in C:
        kk = np.arange(128)
        C["triU"] = (kk[:, None] <= kk[None, :]).astype(np.float32)
        C["triL"] = (kk[:, None] >= kk[None, :]).astype(np.float32)
        C["identf"] = np.eye(128, dtype=np.float32)
        C["blk8"] = np.kron(np.eye(16), np.ones((8, 8))).astype(np.float32)
    x = inp["x"]
    xs = [np.ascontiguousarray(x[c // 4, (c % 4) * TQ:(c % 4 + 1) * TQ]) for c in range(NCORE)]
    R8 = range(NCORE)
    for i in range(n_layers):
        pa = run(prog("PA", build_PA), [pa_inputs(i, inp, c, xs) for c in R8])
        ra = run(prog("A", build_PM_attn, "A"), [dict(attn_inputs_A(pa, c), **_attn_common_dummy(inp, i)) for c in R8])
        rc = run(prog("C", build_PM_attn, "C", TQ, S + 128), [attn_inputs_C(pa, c, inp, i) for c in R8])
        rs = run(prog("SSD", build_SSD), [ssd_inputs(i, inp, pa, c) for c in R8])
        ysd = [np.ascontiguousarray(np.concatenate([np.asarray(rs[(c // 4) * 4 + j]["y"])[(c % 4) * TQ:(c % 4 + 1) * TQ] for j in range(4)], axis=1)) for c in R8]
        pbin = []
        for c in R8:
            pbin.append({"x": xs[c], "oa": np.asarray(ra[c]["o"]), "ys": ysd[c], "z": np.asarray(pa[c]["z"]), "oc": np.asarray(rc[c]["o"]),
                         "od": np.asarray(pa[c]["od"]), "w_gate": inp["w_branch_gate"][i], "w_branch": inp["w_branch"][i].reshape(1024, D),
                         "w_out": inp["w_out"][i], "w_router": np.ascontiguousarray(inp["w_router"][i].reshape(8, 128, 16).transpose(1, 0, 2)),
                         "g_mix": _gl(inp["g_mix"][i]), "g_moe": _gl(inp["g_moe"][i]), "g_ssm": inp["g_ssm"][i],
                         "ident": C["ident"], "identf": C["identf"]})
        del pa, ra, rc, rs
        pb = run(prog("PB", build_PB), pbin)
        del pbin
        thin = []
        for c in R8:
            b = c // 4
            affb = np.concatenate([np.asarray(pb[b * 4 + r]["aff"]) for r in range(4)], axis=0)
            thin.append({"affT": np.ascontiguousarray(affb.T.reshape(128, S // 8)), "blk8": C["blk8"]})
        th = run(prog("TH", build_TH), thin)
        pxin = []
        for c in R8:
            b, r = c // 4, c % 4
            pxin.append({"x1": np.asarray(pb[c]["x1"]), "aff": np.asarray(pb[c]["aff"]),
                         "thr": np.ascontiguousarray(np.asarray(th[c]["thr"]).reshape(16, 8)[:, 0]),
                         "w_exp_gate": inp["w_exp_gate"][i].reshape(16 * D, D), "w_exp_up": inp["w_exp_up"][i].reshape(16 * D, D),
                         "w_exp_down": inp["w_exp_down"][i].reshape(16 * D, D), "g_moe": _gl(inp["g_moe"][i]),
                         "p": np.ascontiguousarray(inp["p"][i][b, r * TQ:(r + 1) * TQ]), "w_ple_gate": inp["w_ple_gate"][i], "w_ple": inp["w_ple"][i],
                         "g_ple": _gl(inp["g_ple"][i]), "ident": C["ident"]})
        del pb
        px = run(prog("PX", build_PX), pxin)
        del pxin
        xs = [np.asarray(px[c]["x3"]) for c in R8]
        del px
    rf = run(prog("F", build_final), [{"x": xs[c], "g_final": inp["g_final"]} for c in R8])
    out = np.zeros((B, S, D), np.float32)
    for c in R8:
        out[c // 4, (c % 4) * TQ:(c % 4 + 1) * TQ] = np.asarray(rf[c]["y"])
    return out


def build_SSD(S_=S):
    k = KB()
    NCH = S_ // 128
    CB_ = min(2048, S_)
    xbc = k.dram("xbc", [128, 3, S_ + 4], BF16, "ExternalInput")
    cwd = k.dram("cw", [128, 3, 5], F32, "ExternalInput")
    cbd = k.dram("cb", [128, 3], F32, "ExternalInput")
    dtrd = k.dram("dtr", [128, NCH, 2], F32, "ExternalInput")
    scd = k.dram("sc", [128, 8], F32, "ExternalInput")
    triUd = k.dram("triU", [128, 128], F32, "ExternalInput")
    triLd = k.dram("triL", [128, 128], F32, "ExternalInput")
    identd = k.dram("identf", [128, 128], F32, "ExternalInput")
    identbd = k.dram("ident", [128, 128], BF16, "ExternalInput")
    yo = k.dram("y", [S_, 64], F32, "ExternalOutput")

    def ld(name, d, shape, dt):
        t = k.sb(name, shape, dt)
        k.dma("sp", t[:], d[tuple(slice(None) for _ in shape)], reads=[d], writes=[t])
        return t
    cw = ld("cw", cwd, [128, 3, 5], F32)
    cb = ld("cb", cbd, [128, 3], F32)
    dtr = ld("dtr", dtrd, [128, NCH, 2], F32)
    sc = ld("sc", scd, [128, 8], F32)
    triU = ld("triU", triUd, [128, 128], F32)
    triL = ld("triL", triLd, [128, 128], F32)
    identf = ld("identf", identd, [128, 128], F32)
    identb = ld("identb", identbd, [128, 128], BF16)
    onesf = k.sb("onesf", [128, 128], F32)
    k.op("dve", lambda en: en.memset(onesf[:], 1.0), writes=[onesf])

    XT = [k.sb("XT%d" % i, [128, S_], BF16) for i in range(3)]
    inb = [k.sb("inb", [128, 3, CB_ + 4], BF16) for _ in range(2)]
    acc = [k.sb("acc", [128, CB_], F32) for _ in range(2)]
    ai = 0
    for bi in range(S_ // CB_):
        ib = inb[bi % 2]
        c0 = bi * CB_
        k.dma("sp", ib[:], xbc[:, :, c0:c0 + CB_ + 4], reads=[xbc], writes=[ib])
        for ti in range(3):
            a = acc[ai % 2]
            ai += 1
            k.op("dve", lambda en, ti=ti: en.tensor_scalar(out=a[:], in0=ib[:, ti, 0:CB_], scalar1=cw[:, ti, 0:1], scalar2=cb[:, ti:ti + 1], op0=ALU.mult, op1=ALU.add),
                 reads=[ib, cw, cb], writes=[a])
            for tp in range(1, 5):
                k.op("dve", lambda en, ti=ti, tp=tp: en.scalar_tensor_tensor(out=a[:], in0=ib[:, ti, tp:tp + CB_], scalar=cw[:, ti, tp:tp + 1], in1=a[:], op0=ALU.mult, op1=ALU.add),
                     reads=[ib, cw, a], writes=[a])
            k.op("act", lambda en, ti=ti: en.activation(out=XT[ti][:, c0:c0 + CB_], in_=a[:], func=AF.Silu), reads=[a], writes=[(XT[ti],)])

    one_t = k.sb("one", [128, 1], F32)
    k.op("dve", lambda en: en.memset(one_t[:], 1.0), writes=[one_t])
    dt = [k.sb("dt", [128, NCH], F32) for _ in range(2)]
    dA = [k.sb("dA", [128, NCH], F32) for _ in range(2)]
    acs = [k.sb("acs", [128, NCH], F32) for _ in range(2)]
    eac = [k.sb("eac", [128, NCH], F32) for _ in range(2)]
    wv = [k.sb("wv", [128, NCH], F32) for _ in range(2)]
    cd = [k.sb("cd", [128, NCH], F32) for _ in range(2)]
    ea = k.sb("ea", [128, 2], F32)
    tmpc = k.sb("tmpc", [128, NCH], F32)
    psm = [k.ps("psm", [128, 512], F32) for _ in range(2)]
    k.op("act", lambda en: en.activation(out=ea[:], in_=sc[:, 2:4], func=AF.Exp), reads=[sc], writes=[ea])
    for d in range(2):
        k.op("act", lambda en, d=d: en.activation(out=tmpc[:], in_=dtr[:, :, d], func=AF.Exp, bias=sc[:, d:d + 1]), reads=[dtr, sc], writes=[tmpc])
        k.op("act", lambda en, d=d: en.activation(out=dt[d][:], in_=tmpc[:], func=AF.Ln, bias=one_t[:, 0:1]), reads=[tmpc, one_t], writes=[dt[d]])
        k.op("dve", lambda en, d=d: en.tensor_scalar(out=dA[d][:], in0=dt[d][:], scalar1=ea[:, d:d + 1], scalar2=-1.0, op0=ALU.mult, op1=ALU.mult),
             reads=[dt[d], ea], writes=[dA[d]])
        tri = triU if d == 0 else triL
        p1, p2 = psm
        k.op("pe", lambda en, d=d, tri=tri: en.matmul(p1[:, 0:NCH], lhsT=tri[:], rhs=dA[d][:], start=True, stop=True), reads=[tri, dA[d]], writes=[p1])
        k.op("pe", lambda en, d=d: en.matmul(p2[:, 0:NCH], lhsT=onesf[:], rhs=dA[d][:], start=True, stop=True), reads=[onesf, dA[d]], writes=[p2])
        k.op("act", lambda en, d=d: en.activation(out=acs[d][:], in_=p1[:, 0:NCH], func=AF.Copy), reads=[p1], writes=[acs[d]])
        k.op("act", lambda en, d=d: en.activation(out=eac[d][:], in_=p1[:, 0:NCH], func=AF.Exp), reads=[p1], writes=[eac[d]])
        k.op("act", lambda en, d=d: en.activation(out=cd[d][:], in_=p2[:, 0:NCH], func=AF.Exp), reads=[p2], writes=[cd[d]])
        k.op("dve", lambda en, d=d: en.tensor_tensor(out=tmpc[:], in0=p2[:, 0:NCH], in1=acs[d][:], op=ALU.subtract), reads=[p2, acs[d]], writes=[tmpc])
        k.op("act", lambda en: en.activation(out=tmpc[:], in_=tmpc[:], func=AF.Exp), reads=[tmpc], writes=[tmpc])
        k.op("dve", lambda en, d=d: en.tensor_tensor(out=wv[d][:], in0=tmpc[:], in1=dt[d][:], op=ALU.mult), reads=[tmpc, dt[d]], writes=[wv[d]])

    psR = [k.ps("psR", [128, 128], F32) for _ in range(2)]
    psCB = k.ps("psCB", [128, 128], F32)
    psT = k.ps("psT", [128, 256], BF16)
    psY = k.ps("psY", [128, 256], F32)
    y1 = k.sb("y1", [128, NCH, 64], F32)
    diag = [k.sb("diag", [128, 128], F32) for _ in range(2)]
    seg = [k.sb("seg", [128, 128], F32) for _ in range(2)]
    Wd = [k.sb("Wd", [128, 128], F32) for _ in range(2)]
    Wt = k.sb("Wt", [128, 128], F32)
    Wb16 = k.sb("Wb16", [128, 128], BF16)
    xs = k.sb("xs", [128, 64], BF16)
    Bs = k.sb("Bs", [128, 128], BF16)
    xw = k.sb("xw", [128, 64], BF16)
    run_ = [k.sb("run", [128, 64], F32) for _ in range(2)]
    prev = k.sb("prev", [128, 64], BF16)
    yst = [k.sb("yst", [128, 8, 64], F32) for _ in range(2)]
    for d in range(2):
        k.op("dve", lambda en, d=d: en.memset(run_[d][:], 0.0), writes=[run_[d]])

    def transposes(c):
        cs_ = slice(c * 128, (c + 1) * 128)
        k.op("pe", lambda en: en.transpose(out=psT[:, 0:64], in_=XT[0][0:64, cs_], identity=identb[0:64, 0:64]), reads=[XT[0], identb], writes=[(psT,)])
        k.op("pe", lambda en: en.transpose(out=psT[:, 128:256], in_=XT[1][:, cs_], identity=identb[:]), reads=[XT[1], identb], writes=[(psT,)])
        k.op("act", lambda en: en.activation(out=xs[:], in_=psT[:, 0:64], func=AF.Copy), reads=[psT], writes=[xs])
        k.op("act", lambda en: en.activation(out=Bs[:], in_=psT[:, 128:256], func=AF.Copy), reads=[psT], writes=[Bs])

    def state_step(c, d):
        cs_ = slice(c * 128, (c + 1) * 128)
        k.op("dve", lambda en: en.tensor_copy(out=prev[:], in_=run_[d][:]), reads=[run_[d]], writes=[prev])
        k.op("pe", lambda en: en.matmul(psY[:, 64:128], lhsT=XT[2][:, cs_], rhs=prev[:], start=True, stop=True), reads=[XT[2], prev], writes=[(psY,)])
        k.op("dve", lambda en: en.tensor_scalar(out=xw[:], in0=xs[:], scalar1=wv[d][:, c:c + 1], scalar2=None, op0=ALU.mult), reads=[xs, wv[d]], writes=[xw])
        k.op("pe", lambda en: en.matmul(psY[:, 128:192], lhsT=Bs[:], rhs=xw[:], start=True, stop=True), reads=[Bs, xw], writes=[(psY,)])
        k.op("dve", lambda en: en.scalar_tensor_tensor(out=run_[d][:], in0=run_[d][:], scalar=cd[d][:, c:c + 1], in1=psY[:, 128:192], op0=ALU.mult, op1=ALU.add),
             reads=[run_[d], cd[d], psY], writes=[run_[d]])

    for c in range(NCH):
        cs_ = slice(c * 128, (c + 1) * 128)
        for d in range(2):
            tri = triU if d == 0 else triL
            k.op("dve", lambda en, d=d: en.tensor_scalar(out=diag[d][:], in0=identf[:], scalar1=acs[d][:, c:c + 1], scalar2=None, op0=ALU.mult),
                 reads=[identf, acs[d]], writes=[diag[d]])
            k.op("pe", lambda en, d=d: en.matmul(psR[d][:], lhsT=onesf[:], rhs=diag[d][:], start=True, stop=True), reads=[onesf, diag[d]], writes=[psR[d]])
            k.op("dve", lambda en, d=d: en.tensor_scalar(out=seg[d][:], in0=psR[d][:], scalar1=acs[d][:, c:c + 1], scalar2=0.0, op0=ALU.subtract, op1=ALU.min),
                 reads=[psR[d], acs[d]], writes=[seg[d]])
            k.op("act", lambda en, d=d: en.activation(out=seg[d][:], in_=seg[d][:], func=AF.Exp), reads=[seg[d]], writes=[seg[d]])
            k.op("dve", lambda en, d=d, tri=tri: en.scalar_tensor_tensor(out=Wd[d][:], in0=seg[d][:], scalar=dt[d][:, c:c + 1], in1=tri[:], op0=ALU.mult, op1=ALU.mult),
                 reads=[seg[d], dt[d], tri], writes=[Wd[d]])
        k.op("pe", lambda en: en.matmul(psCB[:], lhsT=XT[1][:, cs_], rhs=XT[2][:, cs_], start=True, stop=True), reads=[XT[1], XT[2]], writes=[psCB])
        k.op("dve", lambda en: en.tensor_tensor(out=Wt[:], in0=Wd[0][:], in1=Wd[1][:], op=ALU.add), reads=[Wd[0], Wd[1]], writes=[Wt])
        k.op("dve", lambda en: en.tensor_tensor(out=Wt[:], in0=Wt[:], in1=psCB[:], op=ALU.mult), reads=[Wt, psCB], writes=[Wt])
        k.op("dve", lambda en: en.scalar_tensor_tensor(out=Wb16[:], in0=identf[:], scalar=sc[:, 4:5], in1=Wt[:], op0=ALU.mult, op1=ALU.add), reads=[identf, sc, Wt], writes=[Wb16])
        transposes(c)
        k.op("pe", lambda en: en.matmul(psY[:, 0:64], lhsT=Wb16[:], rhs=xs[:], start=True, stop=True), reads=[Wb16, xs], writes=[(psY,)])
        state_step(c, 0)
        k.op("dve", lambda en: en.tensor_copy(out=y1[:, c, :], in_=psY[:, 0:64]), reads=[psY], writes=[(y1,)])
        k.op("dve", lambda en: en.scalar_tensor_tensor(out=y1[:, c, :], in0=psY[:, 64:128], scalar=eac[0][:, c:c + 1], in1=y1[:, c, :], op0=ALU.mult, op1=ALU.add),
             reads=[psY, eac[0], y1], writes=[(y1,)])
    for c in range(NCH - 1, -1, -1):
        transposes(c)
        state_step(c, 1)
        ys = yst[(c // 8) % 2]
        k.op("dve", lambda en: en.scalar_tensor_tensor(out=ys[:, c % 8, :], in0=psY[:, 64:128], scalar=eac[1][:, c:c + 1], in1=y1[:, c, :], op0=ALU.mult, op1=ALU.add),
             reads=[psY, eac[1], y1], writes=[(ys,)])
        if c % 8 == 0:
            nb = min(8, NCH - c)
            k.dma("sp", yo[c * 128:(c + nb) * 128, :].rearrange("(j p) d -> p j d", p=128), ys[:, 0:nb, :], reads=[ys], writes=[(yo,)])
    return k.finish()


def build_PB(TQ=TQ):
    k = KB()
    NB = TQ // 512
    xin = k.dram("x", [TQ, D], F32, "ExternalInput")
    oad = k.dram("oa", [TQ, 256], F32, "ExternalInput")
    ysd = k.dram("ys", [TQ, 256], F32, "ExternalInput")
    zd = k.dram("z", [TQ, 256], F32, "ExternalInput")
    ocd = k.dram("oc", [TQ, 256], F32, "ExternalInput")
    odd = k.dram("od", [TQ, 256], BF16, "ExternalInput")
    wgd = k.dram("w_gate", [D, 4096], F32, "ExternalInput")
    wbd = k.dram("w_branch", [1024, D], F32, "ExternalInput")
    wod = k.dram("w_out", [D, D], F32, "ExternalInput")
    wrd = k.dram("w_router", [128, 8, 16], F32, "ExternalInput")
    gmixd = k.dram("g_mix", [128, 8], F32, "ExternalInput")
    gmoed = k.dram("g_moe", [128, 8], F32, "ExternalInput")
    gssmd = k.dram("g_ssm", [256], F32, "ExternalInput")
    identd = k.dram("ident", [128, 128], BF16, "ExternalInput")
    identfd = k.dram("identf", [128, 128], F32, "ExternalInput")
    x1o = k.dram("x1", [TQ, D], F32, "ExternalOutput")
    affo = k.dram("aff", [TQ, 16], F32, "ExternalOutput")

    def ld(name, d, shape, dt, bc=False):
        t = k.sb(name, shape, dt)
        src = d[:].partition_broadcast(128) if bc else d[tuple(slice(None) for _ in shape)]
        k.dma("sp", t[:], src, reads=[d], writes=[t])
        return t
    ident = ld("ident", identd, [128, 128], BF16)
    identf = ld("identf", identfd, [128, 128], F32)
    gmix = ld("gmix", gmixd, [128, 8], F32)
    gmoe = ld("gmoe", gmoed, [128, 8], F32)
    gssm = ld("gssm", gssmd, [128, 256], F32, bc=True)
    wr = ld("wr", wrd, [128, 8, 16], F32)
    for kc in range(8):
        k.op("dve", lambda en, kc=kc: en.tensor_scalar(out=wr[:, kc, :], in0=wr[:, kc, :], scalar1=gmoe[:, kc:kc + 1], scalar2=None, op0=ALU.mult), reads=[wr, gmoe], writes=[wr])
    eps_t = k.sb("eps", [128, 1], F32)
    k.op("dve", lambda en: en.memset(eps_t[:], EPS), writes=[eps_t])
    stage = [k.sb("wstage", [128, 1024], F32) for _ in range(3)]
    wg = k.sb("wg", [128, 8, 4096], BF16)
    load_cast_weight(k, wgd, 8, 4096, wg, gmix, stage)
    wb = k.sb("wb", [128, 8, 1024], BF16)
    load_cast_weight(k, wbd, 8, 1024, wb, None, stage)
    wo = k.sb("wo", [128, 8, 1024], BF16)
    load_cast_weight(k, wod, 8, 1024, wo, None, stage)

    xt = [k.sb("xt", [128, 4, D], F32) for _ in range(2)]
    hb = k.sb("hb", [128, D], BF16)
    hT = k.sb("hT", [128, 8, 512], BF16)
    brT = k.sb("brT", [128, 8, 512], BF16)
    mT = k.sb("mT", [128, 8, 512], BF16)
    macc = k.sb("macc", [128, 512], F32)
    sig = [k.sb("sig", [128, 512], F32) for _ in range(2)]
    junk = k.sb("junk", [128, D], BF16)
    ss = k.sb("ss", [128, 16], F32)
    br = k.sb("br", [128, 4, 256], BF16)
    bin_ = [k.sb("bin", [128, 4, 256], F32) for _ in range(2)]
    odt = [k.sb("odt", [128, 256], BF16) for _ in range(2)]
    t256 = k.sb("t256", [128, 256], F32)
    hmf = k.sb("hmf", [128, D], F32)
    hmT = k.sb("hmT", [128, 8, 128], F32)
    afft = [k.sb("afft", [128, 16], F32) for _ in range(2)]
    pT = [k.ps("pT", [128, D], BF16) for _ in range(2)]
    pg = [k.ps("pg", [128, 512], F32) for _ in range(2)]
    pw = [k.ps("pw", [128, 512], F32) for _ in range(2)]
    po = [k.ps("po", [128, 512], F32) for _ in range(2)]
    ci = dict(g=0, w=0, o=0, s=0)

    for blk in range(NB):
        xb = xt[blk % 2]
        t0 = blk * 512
        k.dma("sp", xb[:], xin[t0:t0 + 512, :].rearrange("(j p) d -> p j d", p=128), reads=[xin], writes=[xb])
        for j in range(4):
            r0 = t0 + j * 128
            k.op("act", lambda en, j=j: en.activation(out=junk[:], in_=xb[:, j, :], func=AF.Square, accum_out=ss[:, 0:1]), reads=[xb], writes=[junk, ss])
            k.op("act", lambda en: en.activation(out=ss[:, 1:2], in_=ss[:, 0:1], func=AF.Sqrt, bias=eps_t[:, 0:1], scale=1.0 / D), reads=[ss, eps_t], writes=[ss])
            k.op("dve", lambda en: en.reciprocal(out=ss[:, 2:3], in_=ss[:, 1:2]), reads=[ss], writes=[ss])
            k.op("dve", lambda en, j=j: en.tensor_scalar(out=hb[:], in0=xb[:, j, :], scalar1=ss[:, 2:3], scalar2=None, op0=ALU.mult), reads=[xb, ss], writes=[hb])
            pt = pT[0]
            for kc in range(8):
                k.op("pe", lambda en, kc=kc: en.transpose(out=pt[:, kc * 128:(kc + 1) * 128], in_=hb[:, kc * 128:(kc + 1) * 128], identity=ident[:]), reads=[hb, ident], writes=[(pt,)])
            k.op("act", lambda en, j=j: en.activation(out=hT[:, :, j * 128:(j + 1) * 128], in_=pt[:].rearrange("p (c t) -> p c t", c=8), func=AF.Copy), reads=[pt], writes=[(hT,)])
            bi = bin_[j % 2]
            for n_, src in enumerate((oad, ysd, zd, ocd)):
                k.dma("act", bi[:, n_, :], src[r0:r0 + 128, :], reads=[src], writes=[(bi,)])
            ot = odt[j % 2]
            k.dma("act", ot[:], odd[r0:r0 + 128, :], reads=[odd], writes=[ot])
            k.op("act", lambda en: en.activation(out=br[:, 0, :], in_=bi[:, 0, :], func=AF.Copy), reads=[bi], writes=[(br,)])
            k.op("act", lambda en: en.activation(out=br[:, 2, :], in_=bi[:, 3, :], func=AF.Copy), reads=[bi], writes=[(br,)])
            k.op("pool", lambda en: en.tensor_copy(out=br[:, 3, :], in_=ot[:]), reads=[ot], writes=[(br,)])
            k.op("act", lambda en: en.activation(out=t256[:], in_=bi[:, 2, :], func=AF.Silu), reads=[bi], writes=[t256])
            k.op("dve", lambda en: en.tensor_tensor(out=t256[:], in0=t256[:], in1=bi[:, 1, :], op=ALU.mult), reads=[t256, bi], writes=[t256])
            k.op("act", lambda en: en.activation(out=junk[:, 0:256], in_=t256[:], func=AF.Square, accum_out=ss[:, 3:4]), reads=[t256], writes=[junk, ss])
            k.op("act", lambda en: en.activation(out=ss[:, 4:5], in_=ss[:, 3:4], func=AF.Sqrt, bias=eps_t[:, 0:1], scale=1.0 / 256), reads=[ss, eps_t], writes=[ss])
            k.op("dve", lambda en: en.reciprocal(out=ss[:, 5:6], in_=ss[:, 4:5]), reads=[ss], writes=[ss])
            k.op("dve", lambda en: en.scalar_tensor_tensor(out=br[:, 1, :], in0=t256[:], scalar=ss[:, 5:6], in1=gssm[:], op0=ALU.mult, op1=ALU.mult), reads=[t256, ss, gssm], writes=[(br,)])
            pt = pT[1]
            for n_ in range(4):
                for cc in range(2):
                    q_ = n_ * 2 + cc
                    k.op("pe", lambda en, n_=n_, cc=cc, q_=q_: en.transpose(out=pt[:, q_ * 128:(q_ + 1) * 128], in_=br[:, n_, cc * 128:(cc + 1) * 128], identity=ident[:]),
                         reads=[br, ident], writes=[(pt,)])
            k.op("act", lambda en, j=j: en.activation(out=brT[:, :, j * 128:(j + 1) * 128], in_=pt[:].rearrange("p (c t) -> p c t", c=8), func=AF.Copy), reads=[pt], writes=[(brT,)])
        for dc in range(8):
            for n_ in range(4):
                g_ = pg[ci["g"] % 2]
                ci["g"] += 1
                w_ = pw[ci["w"] % 2]
                ci["w"] += 1
                col = n_ * 1024 + dc * 128
                for kc in range(8):
                    k.op("pe", lambda en, kc=kc: en.matmul(g_[:], lhsT=wg[:, kc, col:col + 128], rhs=hT[:, kc, :], start=(kc == 0), stop=(kc == 7)), reads=[wg, hT], writes=[(g_,)])
                for cc in range(2):
                    k.op("pe", lambda en, cc=cc: en.matmul(w_[:], lhsT=wb[:, n_ * 2 + cc, dc * 128:(dc + 1) * 128], rhs=brT[:, n_ * 2 + cc, :], start=(cc == 0), stop=(cc == 1)),
                         reads=[wb, brT], writes=[(w_,)])
                sg_ = sig[ci["s"] % 2]
                ci["s"] += 1
                k.op("act", lambda en: en.activation(out=sg_[:], in_=g_[:], func=AF.Sigmoid), reads=[g_], writes=[sg_])
                if n_ == 0:
                    k.op("dve", lambda en: en.tensor_tensor(out=macc[:], in0=sg_[:], in1=w_[:], op=ALU.mult), reads=[sg_, w_], writes=[macc])
                else:
                    k.op("dve", lambda en: en.tensor_tensor(out=sg_[:], in0=sg_[:], in1=w_[:], op=ALU.mult), reads=[sg_, w_], writes=[sg_])
                    if n_ < 3:
                        k.op("dve", lambda en: en.tensor_tensor(out=macc[:], in0=macc[:], in1=sg_[:], op=ALU.add), reads=[macc, sg_], writes=[macc])
                    else:
                        k.op("dve", lambda en: en.tensor_tensor(out=mT[:, dc, :], in0=macc[:], in1=sg_[:], op=ALU.add), reads=[macc, sg_], writes=[(mT,)])
        for j in range(4):
            r0 = t0 + j * 128
            for hf in range(2):
                p_ = po[ci["o"] % 2]
                ci["o"] += 1
                for dc in range(8):
                    k.op("pe", lambda en, dc=dc: en.matmul(p_[:], lhsT=mT[:, dc, j * 128:(j + 1) * 128], rhs=wo[:, dc, hf * 512:(hf + 1) * 512], start=(dc == 0), stop=(dc == 7)),
                         reads=[mT, wo], writes=[(p_,)])
                k.op("dve", lambda en, hf=hf: en.tensor_tensor(out=xb[:, j, hf * 512:(hf + 1) * 512], in0=xb[:, j, hf * 512:(hf + 1) * 512], in1=p_[:], op=ALU.add), reads=[xb, p_], writes=[(xb,)])
            k.dma("sp", x1o[r0:r0 + 128, :], xb[:, j, :], reads=[xb], writes=[(x1o,)])
            k.op("act", lambda en, j=j: en.activation(out=junk[:], in_=xb[:, j, :], func=AF.Square, accum_out=ss[:, 6:7]), reads=[xb], writes=[junk, ss])
            k.op("act", lambda en: en.activation(out=ss[:, 7:8], in_=ss[:, 6:7], func=AF.Sqrt, bias=eps_t[:, 0:1], scale=1.0 / D), reads=[ss, eps_t], writes=[ss])
            k.op("dve", lambda en: en.reciprocal(out=ss[:, 8:9], in_=ss[:, 7:8]), reads=[ss], writes=[ss])
            k.op("dve", lambda en, j=j: en.tensor_scalar(out=hmf[:], in0=xb[:, j, :], scalar1=ss[:, 8:9], scalar2=None, op0=ALU.mult), reads=[xb, ss], writes=[hmf])
            for hf in range(2):
                p_ = po[ci["o"] % 2]
                ci["o"] += 1
                for q_ in range(4):
                    kc = hf * 4 + q_
                    k.op("pe", lambda en, kc=kc, q_=q_: en.transpose(out=p_[:, q_ * 128:(q_ + 1) * 128], in_=hmf[:, kc * 128:(kc + 1) * 128], identity=identf[:]), reads=[hmf, identf], writes=[(p_,)])
                k.op("act", lambda en, hf=hf: en.activation(out=hmT[:, hf * 4:(hf + 1) * 4, :], in_=p_[:].rearrange("p (c t) -> p c t", c=4), func=AF.Copy), reads=[p_], writes=[(hmT,)])
            p_ = po[ci["o"] % 2]
            ci["o"] += 1
            for kc in range(8):
                k.op("pe", lambda en, kc=kc: en.matmul(p_[:, 0:16], lhsT=hmT[:, kc, :], rhs=wr[:, kc, :], start=(kc == 0), stop=(kc == 7)), reads=[hmT, wr], writes=[(p_,)])
            af = afft[j % 2]
            k.op("dve", lambda en: en.reduce_max(out=ss[:, 9:10], in_=p_[:, 0:16], axis=AX.X), reads=[p_], writes=[ss])
            k.op("dve", lambda en: en.tensor_scalar(out=ss[:, 10:11], in0=ss[:, 9:10], scalar1=-1.0, scalar2=None, op0=ALU.mult), reads=[ss], writes=[ss])
            k.op("act", lambda en: en.activation(out=af[:], in_=p_[:, 0:16], func=AF.Exp, bias=ss[:, 10:11], accum_out=ss[:, 11:12]), reads=[p_, ss], writes=[af, ss])
            k.op("dve", lambda en: en.reciprocal(out=ss[:, 12:13], in_=ss[:, 11:12]), reads=[ss], writes=[ss])
            k.op("dve", lambda en: en.tensor_scalar(out=af[:], in0=af[:], scalar1=ss[:, 12:13], scalar2=None, op0=ALU.mult), reads=[af, ss], writes=[af])
            k.dma("sp", affo[r0:r0 + 128, :], af[:], reads=[af], writes=[(affo,)])
    return k.finish()


def build_TH(S_=S, cap=CAP, iters=40):
    k = KB()
    W = S_ // 8
    affd = k.dram("affT", [128, W], F32, "ExternalInput")
    blkd = k.dram("blk8", [128, 128], F32, "ExternalInput")
    tho = k.dram("thr", [128, 1], F32, "ExternalOutput")
    aff = k.sb("aff", [128, W], F32)
    k.dma("sp", aff[:], affd[:, :], reads=[affd], writes=[aff])
    blk = k.sb("blk", [128, 128], F32)
    k.dma("sp", blk[:], blkd[:, :], reads=[blkd], writes=[blk])
    cmp_ = k.sb("cmp", [128, W], F32)
    v = k.sb("v", [128, 8], F32)
    ps = k.ps("ps", [128, 8], F32)
    k.op("dve", lambda en: en.memset(v[:, 0:1], 0.0), writes=[v])
    k.op("dve", lambda en: en.memset(v[:, 1:2], 1.0), writes=[v])
    for it in range(iters):
        k.op("dve", lambda en: en.tensor_tensor(out=v[:, 2:3], in0=v[:, 0:1], in1=v[:, 1:2], op=ALU.add), reads=[v], writes=[v])
        k.op("dve", lambda en: en.tensor_scalar(out=v[:, 2:3], in0=v[:, 2:3], scalar1=0.5, scalar2=None, op0=ALU.mult), reads=[v], writes=[v])
        k.op("dve", lambda en: en.tensor_scalar(out=cmp_[:], in0=aff[:], scalar1=v[:, 2:3], scalar2=None, op0=ALU.is_ge), reads=[aff, v], writes=[cmp_])
        k.op("dve", lambda en: en.reduce_sum(out=v[:, 3:4], in_=cmp_[:], axis=AX.X), reads=[cmp_], writes=[v])
        k.op("pe", lambda en: en.matmul(ps[:, 0:1], lhsT=blk[:], rhs=v[:, 3:4], start=True, stop=True), reads=[blk, v], writes=[ps])
        k.op("dve", lambda en: en.tensor_scalar(out=v[:, 4:5], in0=ps[:, 0:1], scalar1=float(cap) - 0.5, scalar2=None, op0=ALU.is_ge), reads=[ps], writes=[v])
        k.op("dve", lambda en: en.tensor_tensor(out=v[:, 5:6], in0=v[:, 2:3], in1=v[:, 0:1], op=ALU.subtract), reads=[v], writes=[v])
        k.op("dve", lambda en: en.scalar_tensor_tensor(out=v[:, 0:1], in0=v[:, 5:6], scalar=v[:, 4:5], in1=v[:, 0:1], op0=ALU.mult, op1=ALU.add), reads=[v], writes=[v])
        k.op("dve", lambda en: en.tensor_tensor(out=v[:, 5:6], in0=v[:, 1:2], in1=v[:, 2:3], op=ALU.subtract), reads=[v], writes=[v])
        k.op("dve", lambda en: en.scalar_tensor_tensor(out=v[:, 1:2], in0=v[:, 5:6], scalar=v[:, 4:5], in1=v[:, 2:3], op0=ALU.mult, op1=ALU.add), reads=[v], writes=[v])
    k.dma("sp", tho[:, :], v[:, 0:1], reads=[v], writes=[tho])
    return k.finish()


def build_PX(TQ=TQ, NE=16, TB=1024):
    k = KB()
    NT = TB // 128
    xin = k.dram("x1", [TQ, D], F32, "ExternalInput")
    affd = k.dram("aff", [TQ, 16], F32, "ExternalInput")
    thrd = k.dram("thr", [16], F32, "ExternalInput")
    wgd = k.dram("w_exp_gate", [NE * D, D], F32, "ExternalInput")
    wud = k.dram("w_exp_up", [NE * D, D], F32, "ExternalInput")
    wdd = k.dram("w_exp_down", [NE * D, D], F32, "ExternalInput")
    gmoed = k.dram("g_moe", [128, 8], F32, "ExternalInput")
    pind = k.dram("p", [TQ, 256], F32, "ExternalInput")
    wpgd = k.dram("w_ple_gate", [D, D], F32, "ExternalInput")
    wpld = k.dram("w_ple", [256, D], F32, "ExternalInput")
    gpled = k.dram("g_ple", [128, 8], F32, "ExternalInput")
    identd = k.dram("ident", [128, 128], BF16, "ExternalInput")
    xo = k.dram("x3", [TQ, D], F32, "ExternalOutput")

    ident = k.sb("ident", [128, 128], BF16)
    k.dma("sp", ident[:], identd[:, :], reads=[identd], writes=[ident])
    gmoe = k.sb("gmoe", [128, 8], F32)
    k.dma("sp", gmoe[:], gmoed[:, :], reads=[gmoed], writes=[gmoe])
    gple = k.sb("gple", [128, 8], F32)
    k.dma("sp", gple[:], gpled[:, :], reads=[gpled], writes=[gple])
    thr = k.sb("thr", [128, 16], F32)
    k.dma("sp", thr[:], thrd[:].partition_broadcast(128), reads=[thrd], writes=[thr])
    eps_t = k.sb("eps", [128, 1], F32)
    k.op("dve", lambda en: en.memset(eps_t[:], EPS), writes=[eps_t])
    estage = [k.sb("estage", [128, 1024], F32) for _ in range(3)]
    stage = estage
    wpg = k.sb("wpg", [128, 8, D], BF16)
    load_cast_weight(k, wpgd, 8, D, wpg, gple, stage)
    wpl = k.sb("wpl", [128, 2, D], BF16)
    load_cast_weight(k, wpld, 2, D, wpl, None, stage)
    Wset = [[k.sb("wg", [128, 8, D], BF16), k.sb("wu", [128, 8, D], BF16), k.sb("wd", [128, 8, D], BF16)] for _ in range(2)]

    def load_expert(e_, si):
        w3 = Wset[si]
        load_cast_weight(k, Buf(wgd[e_ * D:(e_ + 1) * D, :], "wgd_e", True), 8, D, w3[0], gmoe, estage, qs=("sp",), cast=("pool",))
        load_cast_weight(k, Buf(wud[e_ * D:(e_ + 1) * D, :], "wud_e", True), 8, D, w3[1], gmoe, estage, qs=("sp",), cast=("pool",))
        load_cast_weight(k, Buf(wdd[e_ * D:(e_ + 1) * D, :], "wdd_e", True), 8, D, w3[2], None, estage, qs=("sp",), cast=("pool",))
    acc = k.sb("acc", [128, NT, D], F32)
    hmT = k.sb("hmT", [128, 8, TB], BF16)
    hb = k.sb("hb", [128, D], BF16)
    junk = k.sb("junk", [128, D], BF16)
    ss = k.sb("ss", [128, 8], F32)
    aft = k.sb("aft", [128, NT, 16], F32)
    wsel = k.sb("wsel", [128, NT, 16], F32)
    hidT = k.sb("hidT", [128, 8, 512], BF16)
    sgb = [k.sb("sgb", [128, 512], F32) for _ in range(2)]
    pt_ = k.sb("pt", [128, 256], F32)
    ptb = k.sb("ptb", [128, 256], BF16)
    pTt = k.sb("pTt", [128, 2, 128], BF16)
    pT = [k.ps("pT", [128, D], BF16) for _ in range(2)]
    pg = [k.ps("pg", [128, 512], F32) for _ in range(2)]
    pu = [k.ps("pu", [128, 512], F32) for _ in range(2)]
    pd = [k.ps("pd", [128, 512], F32) for _ in range(2)]
    ci = dict(g=0, d=0, s=0, t=0)

    def norm_T(t):
        k.op("act", lambda en: en.activation(out=junk[:], in_=acc[:, t, :], func=AF.Square, accum_out=ss[:, 0:1]), reads=[acc], writes=[junk, ss])
        k.op("act", lambda en: en.activation(out=ss[:, 1:2], in_=ss[:, 0:1], func=AF.Sqrt, bias=eps_t[:, 0:1], scale=1.0 / D), reads=[ss, eps_t], writes=[ss])
        k.op("dve", lambda en: en.reciprocal(out=ss[:, 2:3], in_=ss[:, 1:2]), reads=[ss], writes=[ss])
        k.op("dve", lambda en: en.tensor_scalar(out=hb[:], in0=acc[:, t, :], scalar1=ss[:, 2:3], scalar2=None, op0=ALU.mult), reads=[acc, ss], writes=[hb])
        pt = pT[ci["t"] % 2]
        ci["t"] += 1
        for kc in range(8):
            k.op("pe", lambda en, kc=kc: en.transpose(out=pt[:, kc * 128:(kc + 1) * 128], in_=hb[:, kc * 128:(kc + 1) * 128], identity=ident[:]), reads=[hb, ident], writes=[(pt,)])
        k.op("act", lambda en: en.activation(out=hmT[:, :, t * 128:(t + 1) * 128], in_=pt[:].rearrange("p (c t) -> p c t", c=8), func=AF.Copy), reads=[pt], writes=[(hmT,)])

    for blk in range(TQ // TB):
        t0 = blk * TB
        k.dma("sp", acc[:], xin[t0:t0 + TB, :].rearrange("(j p) d -> p j d", p=128), reads=[xin], writes=[acc])
        k.dma("act", aft[:], affd[t0:t0 + TB, :].rearrange("(j p) e -> p j e", p=128), reads=[affd], writes=[aft])
        for t in range(NT):
            norm_T(t)
            k.op("dve", lambda en, t=t: en.tensor_tensor(out=wsel[:, t, :], in0=aft[:, t, :], in1=thr[:], op=ALU.is_ge), reads=[aft, thr], writes=[(wsel,)])
            k.op("dve", lambda en, t=t: en.tensor_tensor(out=wsel[:, t, :], in0=wsel[:, t, :], in1=aft[:, t, :], op=ALU.mult), reads=[wsel, aft], writes=[(wsel,)])
        for e in range(NE):
            gi_ = blk * NE + e
            if gi_ == 0:
                load_expert(0, 0)
            if gi_ + 1 < (TQ // TB) * NE:
                load_expert((e + 1) % NE, (gi_ + 1) % 2)
            wg, wu, wd = Wset[gi_ % 2]
            for sb_ in range(TB // 512):
                c0 = sb_ * 512
                for fc in range(8):
                    g_ = pg[ci["g"] % 2]
                    u_ = pu[ci["g"] % 2]
                    ci["g"] += 1
                    for kc in range(8):
                        k.op("pe", lambda en, kc=kc: en.matmul(g_[:], lhsT=wg[:, kc, fc * 128:(fc + 1) * 128], rhs=hmT[:, kc, c0:c0 + 512], start=(kc == 0), stop=(kc == 7)), reads=[wg, hmT], writes=[(g_,)])
                    for kc in range(8):
                        k.op("pe", lambda en, kc=kc: en.matmul(u_[:], lhsT=wu[:, kc, fc * 128:(fc + 1) * 128], rhs=hmT[:, kc, c0:c0 + 512], start=(kc == 0), stop=(kc == 7)), reads=[wu, hmT], writes=[(u_,)])
                    s_ = sgb[ci["s"] % 2]
                    ci["s"] += 1
                    k.op("act", lambda en: en.activation(out=s_[:], in_=g_[:], func=AF.Silu), reads=[g_], writes=[s_])
                    k.op("dve", lambda en, fc=fc: en.tensor_tensor(out=hidT[:, fc, :], in0=s_[:], in1=u_[:], op=ALU.mult), reads=[s_, u_], writes=[(hidT,)])
                for j in range(4):
                    t = sb_ * 4 + j
                    for hf in range(2):
                        d_ = pd[ci["d"] % 2]
                        ci["d"] += 1
                        for fc in range(8):
                            k.op("pe", lambda en, fc=fc: en.matmul(d_[:], lhsT=hidT[:, fc, j * 128:(j + 1) * 128], rhs=wd[:, fc, hf * 512:(hf + 1) * 512], start=(fc == 0), stop=(fc == 7)), reads=[hidT, wd], writes=[(d_,)])
                        k.op("dve", lambda en, t=t, hf=hf: en.scalar_tensor_tensor(out=acc[:, t, hf * 512:(hf + 1) * 512], in0=d_[:], scalar=wsel[:, t, e:e + 1], in1=acc[:, t, hf * 512:(hf + 1) * 512],
                                                                             op0=ALU.mult, op1=ALU.add), reads=[d_, wsel, acc], writes=[(acc,)])
        for t in range(NT):
            norm_T(t)
        for t in range(NT):
            r0 = t0 + t * 128
            k.dma("act", pt_[:], pind[r0:r0 + 128, :], reads=[pind], writes=[pt_])
            k.op("dve", lambda en: en.tensor_copy(out=ptb[:], in_=pt_[:]), reads=[pt_], writes=[ptb])
            pt = pT[ci["t"] % 2]
            ci["t"] += 1
            for cc in range(2):
                k.op("pe", lambda en, cc=cc: en.transpose(out=pt[:, cc * 128:(cc + 1) * 128], in_=ptb[:, cc * 128:(cc + 1) * 128], identity=ident[:]), reads=[ptb, ident], writes=[(pt,)])
            k.op("act", lambda en: en.activation(out=pTt[:], in_=pt[:, 0:256].rearrange("p (c t) -> p c t", c=2), func=AF.Copy), reads=[pt], writes=[pTt])
            for hf in range(2):
                g_ = pg[ci["g"] % 2]
                u_ = pu[ci["g"] % 2]
                ci["g"] += 1
                for kc in range(8):
                    k.op("pe", lambda en, kc=kc: en.matmul(g_[:], lhsT=hmT[:, kc, t * 128:(t + 1) * 128], rhs=wpg[:, kc, hf * 512:(hf + 1) * 512], start=(kc == 0), stop=(kc == 7)), reads=[hmT, wpg], writes=[(g_,)])
                for cc in range(2):
                    k.op("pe", lambda en, cc=cc: en.matmul(u_[:], lhsT=pTt[:, cc, :], rhs=wpl[:, cc, hf * 512:(hf + 1) * 512], start=(cc == 0), stop=(cc == 1)), reads=[pTt, wpl], writes=[(u_,)])
                s_ = sgb[ci["s"] % 2]
                ci["s"] += 1
                k.op("act", lambda en: en.activation(out=s_[:], in_=g_[:], func=AF.Sigmoid), reads=[g_], writes=[s_])
                k.op("dve", lambda en: en.tensor_tensor(out=s_[:], in0=s_[:], in1=u_[:], op=ALU.mult), reads=[s_, u_], writes=[s_])
                k.op("dve", lambda en, t=t, hf=hf: en.tensor_tensor(out=acc[:, t, hf * 512:(hf + 1) * 512], in0=acc[:, t, hf * 512:(hf + 1) * 512], in1=s_[:], op=ALU.add), reads=[acc, s_], writes=[(acc,)])
        k.dma("sp", xo[t0:t0 + TB, :].rearrange("(j p) d -> p j d", p=128), acc[:], reads=[acc], writes=[(xo,)])
    return k.finish()
```

```python
import math
from contextlib import ExitStack

import numpy as np
import ml_dtypes
import concourse.bass as bass
import concourse.mybir as mybir
from concourse.bass_utils import run_bass_kernel_spmd

F32 = mybir.dt.float32
BF16 = mybir.dt.bfloat16
I32 = mybir.dt.int32
AF = mybir.ActivationFunctionType
ALU = mybir.AluOpType
AX = mybir.AxisListType
NPBF = ml_dtypes.bfloat16

D = 1024
B = 2
S = 16384
DEPTH = 4
NCORE = 8
TQ = S // 4
EPS = 1e-6
CAP = 2048


class Buf:
    __slots__ = ("t", "name", "w", "r", "war", "dsem", "is_dram", "is_psum")

    def __init__(self, t, name, is_dram=False):
        self.is_dram = is_dram
        self.is_psum = False
        self.t = t
        self.name = name
        self.w = {}
        self.r = {}
        self.war = {}
        self.dsem = None

    def __getitem__(self, idx):
        return self.t[idx]


class DSem:
    __slots__ = ("sem", "cnt")

    def __init__(self, sem):
        self.sem = sem
        self.cnt = 0


class KB:
    def __init__(self):
        self.nc = bass.Bass("TRN2", target_bir_lowering=False)
        self.es = ExitStack()
        nc = self.nc
        self.eng = dict(pe=nc.tensor, dve=nc.vector, act=nc.scalar, pool=nc.gpsimd, sp=nc.sync)
        self.esem = {}
        self.ecnt = {}
        self.known = {}
        self.sems = {}
        for e in self.eng:
            s = self.es.enter_context(nc.semaphore("es_" + e))
            self.esem[e] = s
            self.ecnt[e] = 0
            self.known[e] = {}
            self.sems[id(s)] = s
        self.dsems = []
        self.nbuf = 0
        self.out_tokens = []

    def sb(self, name, shape, dt):
        self.nbuf += 1
        t = self.es.enter_context(self.nc.sbuf_tensor(f"{name}_{self.nbuf}", list(shape), dt))
        return Buf(t, name)

    def ps(self, name, shape, dt):
        self.nbuf += 1
        t = self.es.enter_context(self.nc.psum_tensor(f"{name}_{self.nbuf}", list(shape), dt))
        b = Buf(t, name)
        b.is_psum = True
        return b

    def dram(self, name, shape, dt, kind):
        t = self.nc.dram_tensor(name, list(shape), dt, kind=kind)
        b = Buf(t.ap(), name, True)
        return b

    def new_dsem(self, name):
        self.nbuf += 1
        s = self.es.enter_context(self.nc.semaphore(f"ds_{name}_{self.nbuf}"))
        d = DSem(s)
        self.sems[id(s)] = s
        self.dsems.append(d)
        return d

    def _wait(self, e, need):
        kn = self.known[e]
        engine = self.eng[e]
        for sid, val in need.items():
            if e == "pe" and sid == id(self.esem["pe"]):
                continue
            if kn.get(sid, 0) < val:
                engine.wait_ge(self.sems[sid], val)
                kn[sid] = val

    @staticmethod
    def _merge(dst, src):
        for k, v in src.items():
            if dst.get(k, 0) < v:
                dst[k] = v

    def _hazards(self, reads, writes):
        need = {}
        for b in reads:
            self._merge(need, b.w)
            if b.is_psum:
                self._merge(need, b.r)
        for w in writes:
            part = False
            if isinstance(w, tuple):
                w, part = w[0], True
            if w.r:
                w.war = dict(w.r)
                w.r = {}
                if part:
                    w.w = {}
            self._merge(need, w.war)
            if not part:
                self._merge(need, w.w)
        return need

    def _record(self, reads, writes, sid, val):
        for b in reads:
            if b.r.get(sid, 0) < val:
                b.r[sid] = val
        for w in writes:
            if isinstance(w, tuple):
                w = w[0]
                if w.w.get(sid, 0) < val:
                    w.w[sid] = val
            else:
                w.w = {sid: val}

    def op(self, e, fn, reads=(), writes=()):
        need = self._hazards(reads, writes)
        self._wait(e, need)
        ins = fn(self.eng[e])
        self.ecnt[e] += 1
        ins.then_inc(self.esem[e], 1)
        self._record(reads, writes, id(self.esem[e]), self.ecnt[e])
        return ins

    def dma(self, q, out_ap, in_ap, reads=(), writes=(), dsem=None, is_output=False, **kw):
        need = self._hazards(reads, writes)
        self._wait(q, need)
        if dsem is None:
            for b in list(writes) + list(reads):
                bb = b[0] if isinstance(b, tuple) else b
                if bb.is_dram:
                    continue
                if bb.dsem is None:
                    bb.dsem = self.new_dsem(bb.name)
                dsem = bb.dsem
                break
        assert dsem is not None
        ins = self.eng[q].dma_start(out=out_ap, in_=in_ap, **kw)
        dsem.cnt += 16
        ins.then_inc(dsem.sem, 16)
        self._record(reads, writes, id(dsem.sem), dsem.cnt)
        return ins

    def idma(self, fn, reads=(), writes=(), dsem=None):
        need = self._hazards(reads, writes)
        self._wait("pool", need)
        ins = fn(self.eng["pool"])
        dsem.cnt += 16
        ins.then_inc(dsem.sem, 16)
        self._record(reads, writes, id(dsem.sem), dsem.cnt)
        return ins

    def finish(self):
        need = {}
        for e in self.eng:
            if e != "sp":
                need[id(self.esem[e])] = self.ecnt[e]
        for d in self.dsems:
            need[id(d.sem)] = d.cnt
        need = {k: v for k, v in need.items() if v > 0}
        self._wait("sp", need)
        self.es.close()
        return self.nc


def _bc(ap_1d_dram, n, parts=128):
    return ap_1d_dram.partition_broadcast(parts) if hasattr(ap_1d_dram, "partition_broadcast") else ap_1d_dram


_WIN_SEGS = [
    (0, 256, 0),
    (256, 384, 256),
    (768, 1536, 384),
    (1544, 1800, 1152),
    (1800, 2056, 1408),
    (1536, 1544, 1664),
    (384, 512, 1672),
    (512, 768, 1800),
    (2056, 2312, 2056),
    (2312, 2824, 2312),
]
C_QA, C_KA, C_XBC, C_CQ, C_CK, C_DT, C_VA, C_Z, C_CV, C_UV = 0, 256, 384, 1152, 1408, 1664, 1672, 1800, 2056, 2312


def load_cast_weight(k, w_dram, kchunks, ncols, wbf, g_sb, stage_bufs, segs=None, qs=("sp", "act"), cast=("pool", "dve")):
    if segs is None:
        segs = [(0, ncols, 0)]
    i = 0
    for kc in range(kchunks):
        for (s0, s1, d0) in segs:
            n = s1 - s0
            for c0 in range(0, n, 1024):
                c1 = min(n, c0 + 1024)
                st = stage_bufs[i % len(stage_bufs)]
                q = qs[i % len(qs)]
                k.dma(q, st[:, 0:c1 - c0], w_dram[kc * 128:(kc + 1) * 128, s0 + c0:s0 + c1], reads=[w_dram], writes=[st])
                e = cast[i % len(cast)]
                dst = wbf[:, kc, d0 + c0:d0 + c1]
                src = st[:, 0:c1 - c0]
                if g_sb is not None:
                    k.op(e, lambda en, dst=dst, src=src, kc=kc: en.tensor_scalar(out=dst, in0=src, scalar1=g_sb[:, kc:kc + 1], scalar2=None, op0=ALU.mult),
                         reads=[st, g_sb], writes=[(wbf,)])
                else:
                    k.op(e, lambda en, dst=dst, src=src: en.tensor_copy(out=dst, in_=src), reads=[st], writes=[(wbf,)])
                i += 1


PA_SKIP = set()


def build_PA(prefix=False, final=False, TQ=TQ):
    k = KB()
    nc = k.nc
    NB = TQ // 512
    xin = k.dram("x", [TQ, D], F32, "ExternalInput")
    if prefix:
        parts = k.dram("parts", [4, TQ, D], F32, "ExternalInput")
        pin = k.dram("p", [TQ, 256], F32, "ExternalInput")
        wpg = k.dram("w_ple_gate", [D, D], F32, "ExternalInput")
        wpl = k.dram("w_ple", [256, D], F32, "ExternalInput")
        gple = k.dram("g_ple", [128, 8], F32, "ExternalInput")
    if final:
        gfin = k.dram("g_final", [D], F32, "ExternalInput")
        yout = k.dram("y", [TQ, D], F32, "ExternalOutput")
    else:
        win = k.dram("w_in", [D, 2824], F32, "ExternalInput")
        gmix = k.dram("g_mix", [128, 8], F32, "ExternalInput")
        gqk = k.dram("gqk", [128, 4], F32, "ExternalInput")
        ctd = k.dram("rope_c", [128, TQ], F32, "ExternalInput")
        std = k.dram("rope_s", [128, TQ], F32, "ExternalInput")
        permd = k.dram("perm", [128, 128], BF16, "ExternalInput")
        onesd = k.dram("onesblk", [128, 128], F32, "ExternalInput")
        gsgud = k.dram("g_sgu", [256], F32, "ExternalInput")
        wsTd = k.dram("wsT", [128, 4, 128], F32, "ExternalInput")
        bspd = k.dram("bsp", [128, 4], F32, "ExternalInput")
        o_qAT = k.dram("qAT", [256, TQ], BF16, "ExternalOutput")
        o_kAT = k.dram("kAT", [128, TQ], BF16, "ExternalOutput")
        o_vA = k.dram("vA", [TQ, 128], BF16, "ExternalOutput")
        o_qCT = k.dram("qCT", [256, TQ], BF16, "ExternalOutput")
        o_kCT = k.dram("kCT", [256, TQ], BF16, "ExternalOutput")
        o_vC = k.dram("vC", [TQ, 256], BF16, "ExternalOutput")
        o_z = k.dram("z", [TQ, 256], F32, "ExternalOutput")
        o_xbcT = k.dram("xbcT", [768, TQ], BF16, "ExternalOutput")
        o_dtT = k.dram("dtT", [8, TQ], F32, "ExternalOutput")
        o_od = k.dram("od", [TQ, 256], BF16, "ExternalOutput")
    if prefix and not final:
        o_x = k.dram("xo", [TQ, D], F32, "ExternalOutput")
    identd = k.dram("ident", [128, 128], BF16, "ExternalInput")

    ident = k.sb("ident", [128, 128], BF16)
    k.dma("sp", ident[:], identd[:, :], reads=[identd], writes=[ident])
    stage = [k.sb("wstage", [128, 1024], F32) for _ in range(3)]
    eps_t = k.sb("eps", [128, 1], F32)
    k.op("dve", lambda en: en.memset(eps_t[:], EPS), writes=[eps_t])

    xt = [k.sb("xt", [128, 4, D], F32) for _ in range(2)]
    hb = k.sb("hb", [128, D], BF16)
    hT = [k.sb("hT", [128, 8, 512], BF16) for _ in range(2)]
    junk = k.sb("junk", [128, D], BF16)
    ss = k.sb("ss", [128, 8], F32)
    pT = [k.ps("pT", [128, D], BF16) for _ in range(2)]
    pb = [k.ps("pb", [128, 512], F32) for _ in range(6)]
    pbi = [0]

    def bank():
        b = pb[pbi[0] % len(pb)]
        pbi[0] += 1
        return b

    def rms_to_hT(xsrc_ap, xbuf, hT_b, j, g_unused=None):
        k.op("act", lambda en: en.activation(out=junk[:], in_=xsrc_ap, func=AF.Square, accum_out=ss[:, 0:1]),
             reads=[xbuf], writes=[junk, ss])
        k.op("act", lambda en: en.activation(out=ss[:, 1:2], in_=ss[:, 0:1], func=AF.Sqrt, bias=eps_t[:, 0:1], scale=1.0 / D),
             reads=[ss, eps_t], writes=[ss])
        k.op("dve", lambda en: en.reciprocal(out=ss[:, 2:3], in_=ss[:, 1:2]), reads=[ss], writes=[ss])
        k.op("dve", lambda en: en.tensor_scalar(out=hb[:], in0=xsrc_ap, scalar1=ss[:, 2:3], scalar2=None, op0=ALU.mult),
             reads=[xbuf, ss], writes=[hb])
        pt = pT[j % 2]
        for kc in range(8):
            k.op("pe", lambda en, kc=kc: en.transpose(out=pt[:, kc * 128:(kc + 1) * 128], in_=hb[:, kc * 128:(kc + 1) * 128], identity=ident[:]),
                 reads=[hb, ident], writes=[(pt,)])
        k.op("act", lambda en: en.activation(out=hT_b[:, :, j * 128:(j + 1) * 128], in_=pt[:].rearrange("p (c t) -> p c t", c=8), func=AF.Copy),
             reads=[pt], writes=[(hT_b,)])

    if not final:
        wbf = k.sb("wbf", [128, 8, 2824], BF16)
        gmix_sb = k.sb("gmix", [128, 8], F32)
        k.dma("sp", gmix_sb[:], gmix[:, :], reads=[gmix], writes=[gmix_sb])
        load_cast_weight(k, win, 8, 2824, wbf, gmix_sb, stage, segs=_WIN_SEGS)
        gqk_sb = k.sb("gqk", [128, 4], F32)
        k.dma("sp", gqk_sb[:], gqk[:, :], reads=[gqk], writes=[gqk_sb])
        perm = k.sb("perm", [128, 128], BF16)
        k.dma("sp", perm[:], permd[:, :], reads=[permd], writes=[perm])
        onesb = k.sb("onesb", [128, 128], F32)
        k.dma("sp", onesb[:], onesd[:, :], reads=[onesd], writes=[onesb])
        gsgu = k.sb("gsgu", [128, 256], F32)
        k.dma("sp", gsgu[:], gsgud[:].partition_broadcast(128), reads=[gsgud], writes=[gsgu])
        wsf = k.sb("wsf", [128, 4, 128], F32)
        k.dma("sp", wsf[:], wsTd[:, :, :], reads=[wsTd], writes=[wsf])
        wsb = k.sb("wsb", [128, 4, 128], BF16)
        k.op("dve", lambda en: en.tensor_copy(out=wsb[:], in_=wsf[:]), reads=[wsf], writes=[wsb])
        bsp = k.sb("bsp", [128, 4], F32)
        k.dma("sp", bsp[:], bspd[:, :], reads=[bspd], writes=[bsp])
        ct = [k.sb("ct", [128, 512], F32) for _ in range(2)]
        stb = [k.sb("st", [128, 512], F32) for _ in range(2)]
        sg_fm = [k.sb("sg_fm", [128, 512], BF16) for _ in range(4)]
        sg_dt = [k.sb("sg_dt", [8, 512], F32) for _ in range(2)]
        sg_vA = [k.sb("sg_vA", [128, 128], BF16) for _ in range(2)]
        sg_z = [k.sb("sg_z", [128, 256], F32) for _ in range(2)]
        sg_vC = [k.sb("sg_vC", [128, 256], BF16) for _ in range(2)]
        sg_od = [k.sb("sg_od", [128, 256], BF16) for _ in range(2)]
        sq = k.sb("sq", [128, 512], F32)
        rstd = k.sb("rstd", [128, 512], F32)
        qg = k.sb("qg", [128, 512], BF16)
        t1 = k.sb("t1", [128, 512], F32)
        t2 = k.sb("t2", [128, 512], F32)
        gel = k.sb("gel", [128, 512], F32)
        vn = k.sb("vn", [128, 256], BF16)
        fmi = [0]

        for blk in range(NB):
            xb = xt[blk % 2]
            hTb = hT[blk % 2]
            t0 = blk * 512
            k.dma("sp", xb[:], xin[t0:t0 + 512, :].rearrange("(j p) d -> p j d", p=128), reads=[xin], writes=[xb])
            k.dma("act", ct[blk % 2][:], ctd[:, t0:t0 + 512], reads=[ctd], writes=[ct[blk % 2]])
            k.dma("act", stb[blk % 2][:], std[:, t0:t0 + 512], reads=[std], writes=[stb[blk % 2]])
            for j in range(4):
                rms_to_hT(xb[:, j, :], xb, hTb, j)

            def fm_chunk(col0, m=128):
                ps = bank()
                for kc in range(8):
                    k.op("pe", lambda en, kc=kc: en.matmul(ps[0:m, :], lhsT=wbf[:, kc, col0:col0 + m], rhs=hTb[:, kc, :], start=(kc == 0), stop=(kc == 7)),
                         reads=[wbf, hTb], writes=[(ps,)])
                return ps

            def fm_out(src_ps, dram, row0):
                sg = sg_fm[fmi[0] % 4]
                fmi[0] += 1
                e = ("act", "dve")[fmi[0] % 2]
                if e == "act":
                    k.op("act", lambda en: en.activation(out=sg[:], in_=src_ps[:], func=AF.Copy), reads=[src_ps], writes=[sg])
                else:
                    k.op("dve", lambda en: en.tensor_copy(out=sg[:], in_=src_ps[:]), reads=[src_ps], writes=[sg])
                k.dma("sp", dram[row0:row0 + 128, t0:t0 + 512], sg[:], reads=[sg], writes=[(dram,)])

            for (col0, dram, row0, gi) in ((C_QA, o_qAT, 0, 0), (C_QA + 128, o_qAT, 128, 0), (C_KA, o_kAT, 0, 2)) if 'qk' not in PA_SKIP else ():
                ps = fm_chunk(col0)
                k.op("act", lambda en: en.activation(out=sq[:], in_=ps[:], func=AF.Square), reads=[ps], writes=[sq])
                ps2 = bank()
                k.op("pe", lambda en: en.matmul(ps2[:], lhsT=onesb[:], rhs=sq[:], start=True, stop=True), reads=[onesb, sq], writes=[ps2])
                k.op("act", lambda en: en.activation(out=rstd[:], in_=ps2[:], func=AF.Sqrt, bias=eps_t[:, 0:1], scale=1.0), reads=[ps2, eps_t], writes=[rstd])
                k.op("dve", lambda en: en.reciprocal(out=rstd[:], in_=rstd[:]), reads=[rstd], writes=[rstd])
                k.op("dve", lambda en: en.scalar_tensor_tensor(out=qg[:], in0=ps[:], scalar=gqk_sb[:, gi:gi + 1], in1=rstd[:], op0=ALU.mult, op1=ALU.mult),
                     reads=[ps, gqk_sb, rstd], writes=[qg])
                ps3 = bank()
                k.op("pe", lambda en: en.matmul(ps3[:], lhsT=perm[:], rhs=qg[:], start=True, stop=True), reads=[perm, qg], writes=[ps3])
                k.op("dve", lambda en: en.tensor_tensor(out=t1[:], in0=qg[:], in1=ct[blk % 2][:], op=ALU.mult), reads=[qg, ct[blk % 2]], writes=[t1])
                k.op("dve", lambda en: en.tensor_tensor(out=t2[:], in0=ps3[:], in1=stb[blk % 2][:], op=ALU.mult), reads=[ps3, stb[blk % 2]], writes=[t2])
                sg = sg_fm[fmi[0] % 4]
                fmi[0] += 1
                k.op("dve", lambda en: en.tensor_tensor(out=sg[:], in0=t1[:], in1=t2[:], op=ALU.add), reads=[t1, t2], writes=[sg])
                k.dma("sp", dram[row0:row0 + 128, t0:t0 + 512], sg[:], reads=[sg], writes=[(dram,)])
            for c in range(6 if 'fm' not in PA_SKIP else 0):
                fm_out(fm_chunk(C_XBC + c * 128), o_xbcT, c * 128)
            for c in range(2):
                fm_out(fm_chunk(C_CQ + c * 128), o_qCT, c * 128)
            for c in range(2):
                fm_out(fm_chunk(C_CK + c * 128), o_kCT, c * 128)
            ps = fm_chunk(C_DT, m=8)
            sd = sg_dt[blk % 2]
            k.op("act", lambda en: en.activation(out=sd[:], in_=ps[0:8, :], func=AF.Copy), reads=[ps], writes=[sd])
            k.dma("sp", o_dtT[:, t0:t0 + 512], sd[:], reads=[sd], writes=[(o_dtT,)])

            for j in range(4 if 'tm' not in PA_SKIP else 0):
                r0 = t0 + j * 128

                def tm_group(col0, n):
                    ps = bank()
                    for kc in range(8):
                        k.op("pe", lambda en, kc=kc: en.matmul(ps[:, 0:n], lhsT=hTb[:, kc, j * 128:(j + 1) * 128], rhs=wbf[:, kc, col0:col0 + n], start=(kc == 0), stop=(kc == 7)),
                             reads=[wbf, hTb], writes=[(ps,)])
                    return ps
                if 'va' not in PA_SKIP:
                    ps = tm_group(C_VA, 128)
                    sv = sg_vA[j % 2]
                    k.op("act", lambda en: en.activation(out=sv[:], in_=ps[:, 0:128], func=AF.Copy), reads=[ps], writes=[sv])
                    k.dma("sp", o_vA[r0:r0 + 128, :], sv[:], reads=[sv], writes=[(o_vA,)])
                if 'zc' not in PA_SKIP:
                    ps = tm_group(C_Z, 512)
                    sz = sg_z[j % 2]
                    k.op("act", lambda en: en.activation(out=sz[:], in_=ps[:, 0:256], func=AF.Copy), reads=[ps], writes=[sz])
                    k.dma("sp", o_z[r0:r0 + 128, :], sz[:], reads=[sz], writes=[(o_z,)])
                    sc = sg_vC[j % 2]
                    k.op("act", lambda en: en.activation(out=sc[:], in_=ps[:, 256:512], func=AF.Copy), reads=[ps], writes=[sc])
                    k.dma("sp", o_vC[r0:r0 + 128, :], sc[:], reads=[sc], writes=[(o_vC,)])
                if 'sgu' in PA_SKIP:
                    continue
                ps = tm_group(C_UV, 512)
                k.op("act", lambda en: en.activation(out=gel[:], in_=ps[:], func=AF.Gelu_apprx_tanh), reads=[ps], writes=[gel])
                k.op("act", lambda en: en.activation(out=junk[:, 0:256], in_=gel[:, 256:512], func=AF.Square, accum_out=ss[:, 4:5]), reads=[gel], writes=[junk, ss])
                k.op("act", lambda en: en.activation(out=ss[:, 5:6], in_=ss[:, 4:5], func=AF.Sqrt, bias=eps_t[:, 0:1], scale=1.0 / 256), reads=[ss, eps_t], writes=[ss])
                k.op("dve", lambda en: en.reciprocal(out=ss[:, 6:7], in_=ss[:, 5:6]), reads=[ss], writes=[ss])
                k.op("dve", lambda en: en.scalar_tensor_tensor(out=vn[:], in0=gel[:, 256:512], scalar=ss[:, 6:7], in1=gsgu[:], op0=ALU.mult, op1=ALU.mult),
                     reads=[gel, ss, gsgu], writes=[vn])
                ps2 = bank()
                for g in range(4):
                    k.op("pe", lambda en, g=g: en.matmul(ps2[:, g * 64:(g + 1) * 64], lhsT=wsb[:, g, :], rhs=vn[:, g * 64:(g + 1) * 64], start=True, stop=True),
                         reads=[wsb, vn], writes=[(ps2,)])
                so = sg_od[j % 2]
                for g in range(4):
                    k.op("dve", lambda en, g=g: en.scalar_tensor_tensor(out=so[:, g * 64:(g + 1) * 64], in0=ps2[:, g * 64:(g + 1) * 64], scalar=bsp[:, g:g + 1],
                                                                       in1=gel[:, g * 64:(g + 1) * 64], op0=ALU.add, op1=ALU.mult),
                         reads=[ps2, bsp, gel], writes=[(so,)])
                k.dma("sp", o_od[r0:r0 + 128, :], so[:], reads=[so], writes=[(o_od,)])
    return k.finish()


def _rope_tables():
    t = np.arange(S)
    row = (t // 64).astype(np.float32)
    col = (t % 64).astype(np.float32)
    freqs = (10000.0 ** (-np.arange(0, 32, 2, dtype=np.float32) / 32)).astype(np.float32)
    ct = np.zeros((128, S), np.float32)
    st = np.zeros((128, S), np.float32)
    perm = np.zeros((128, 128), np.float32)
    for p in range(128):
        d = p % 64
        half = d // 32
        w = d % 32
        m = w % 16
        pos = row if half == 0 else col
        ang = pos * freqs[m]
        ct[p] = np.cos(ang)
        st[p] = -np.sin(ang) if w < 16 else np.sin(ang)
        partner = p + 16 if w < 16 else p - 16
        perm[partner, p] = 1.0
    return ct, st, perm


def _partner_perm_vec(g64):
    g = np.concatenate([g64, g64]).astype(np.float32)
    out = np.zeros(128, np.float32)
    for p in range(128):
        w = (p % 64) % 32
        partner = p + 16 if w < 16 else p - 16
        out[p] = g[partner]
    return g, out


_CONST = {}


def consts():
    if not _CONST:
        ct, st, perm = _rope_tables()
        _CONST["ct"], _CONST["st"] = ct, st
        _CONST["perm"] = perm.astype(NPBF)
        _CONST["ident"] = np.eye(128, dtype=np.float32).astype(NPBF)
        ob = np.zeros((128, 128), np.float32)
        ob[:64, :64] = 1.0 / 64
        ob[64:, 64:] = 1.0 / 64
        _CONST["onesblk"] = ob
    return _CONST


_PROG = {}


def prog(name, fn, *a):
    key = (name,) + a
    if key not in _PROG:
        _PROG[key] = fn(*a)
    return _PROG[key]


def run(nc, in_maps):
    res = run_bass_kernel_spmd(nc, in_maps, core_ids=list(range(NCORE)))
    return res.results


def pa_inputs(i, inp, c, xs):
    C = consts()
    b, r = c // 4, c % 4
    t0 = r * TQ
    gq, gqr = _partner_perm_vec(inp["g_qnorm"][i])
    gk, gkr = _partner_perm_vec(inp["g_knorm"][i])
    return {
        "x": xs[c],
        "w_in": inp["w_in"][i],
        "g_mix": np.ascontiguousarray(inp["g_mix"][i].reshape(8, 128).T),
        "gqk": np.ascontiguousarray(np.stack([gq, gqr, gk, gkr], axis=1)),
        "rope_c": np.ascontiguousarray(C["ct"][:, t0:t0 + TQ]),
        "rope_s": np.ascontiguousarray(C["st"][:, t0:t0 + TQ]),
        "perm": C["perm"], "onesblk": C["onesblk"], "ident": C["ident"],
        "g_sgu": inp["g_sgu"][i],
        "wsT": np.ascontiguousarray(inp["w_spatial"][i].transpose(2, 0, 1)),
        "bsp": np.ascontiguousarray(inp["b_spatial"][i].T),
    }


def _t5_bucket(rel):
    nb, max_exact = 16, 8
    ret = np.where(rel > 0, nb, 0)
    r = np.abs(rel)
    rf = np.maximum(r, 1).astype(np.float32)
    large = max_exact + (np.log(rf / max_exact) / np.float32(math.log(128 / max_exact)) * (nb - max_exact)).astype(np.int32)
    large = np.minimum(large, nb - 1)
    return ret + np.where(r < max_exact, r, large)


def _bias_bucket_strip():
    out = np.zeros((128, 9 * 128), np.float32)
    kk = np.arange(128)[:, None]
    qq = np.arange(128)[None, :]
    for c in range(9):
        dlt = 4 - c
        out[:, c * 128:(c + 1) * 128] = _t5_bucket(128 * dlt + kk - qq).astype(np.float32)
    return out


def build_PM_attn(mode, NQ=TQ, NK=S):
    k = KB()
    NKT = NK // 128
    NQB = NQ // 512
    if mode == "A":
        qA = k.dram("qA", [128, 2, NQ], BF16, "ExternalInput")
        kA = k.dram("kA", [128, NK], BF16, "ExternalInput")
        vA = k.dram("vA", [NK, 2, 65], BF16, "ExternalInput")
    else:
        qC = k.dram("qC", [128, 2, 2, NQ], BF16, "ExternalInput")
        kC = k.dram("kC", [128, 2, NK], BF16, "ExternalInput")
        vC = k.dram("vC", [NK, 4, 65], BF16, "ExternalInput")
    bidxd = k.dram("bidx", [128, 9 * 128], F32, "ExternalInput")
    relbd = k.dram("relb", [128], F32, "ExternalInput")
    lamvd = k.dram("lamv", [128], F32, "ExternalInput")
    lamid = k.dram("lami", [2], F32, "ExternalInput")
    gdifd = k.dram("gdiff", [64], F32, "ExternalInput")
    oa = k.dram("o", [NQ, 256], F32, "ExternalOutput")

    relb = k.sb("relb", [128, 128], F32)
    k.dma("sp", relb[:], relbd[:].partition_broadcast(128), reads=[relbd], writes=[relb])
    lamv = k.sb("lamv", [128, 128], F32)
    k.dma("sp", lamv[:], lamvd[:].partition_broadcast(128), reads=[lamvd], writes=[lamv])
    lami = k.sb("lami", [128, 2], F32)
    k.dma("sp", lami[:], lamid[:].partition_broadcast(128), reads=[lamid], writes=[lami])
    gdif = k.sb("gdif", [128, 64], F32)
    k.dma("sp", gdif[:], gdifd[:].partition_broadcast(128), reads=[gdifd], writes=[gdif])
    eps_t = k.sb("eps", [128, 1], F32)
    k.op("dve", lambda en: en.memset(eps_t[:], EPS), writes=[eps_t])
    if mode == "C":
        tbd = k.dram("tb", [4 * NKT], F32, "ExternalInput")
        tb = k.sb("tb", [128, 4 * NKT], F32)
        k.dma("sp", tb[:], tbd[:].partition_broadcast(128), reads=[tbd], writes=[tb])
    sm = k.sb("sm", [128, 16], F32)
    tmp = k.sb("tmp", [128, 64], F32)
    k.op("dve", lambda en: en.tensor_tensor(out=tmp[:, 0:32], in0=lamv[:, 0:32], in1=lamv[:, 32:64], op=ALU.mult), reads=[lamv], writes=[tmp])
    k.op("dve", lambda en: en.tensor_tensor(out=tmp[:, 32:64], in0=lamv[:, 64:96], in1=lamv[:, 96:128], op=ALU.mult), reads=[lamv], writes=[tmp])
    k.op("dve", lambda en: en.reduce_sum(out=sm[:, 0:1], in_=tmp[:, 0:32], axis=AX.X), reads=[tmp], writes=[sm])
    k.op("dve", lambda en: en.reduce_sum(out=sm[:, 1:2], in_=tmp[:, 32:64], axis=AX.X), reads=[tmp], writes=[sm])
    k.op("act", lambda en: en.activation(out=sm[:, 2:4], in_=sm[:, 0:2], func=AF.Exp), reads=[sm], writes=[sm])
    k.op("dve", lambda en: en.tensor_tensor(out=sm[:, 4:5], in0=sm[:, 2:3], in1=sm[:, 3:4], op=ALU.subtract), reads=[sm], writes=[sm])
    k.op("dve", lambda en: en.tensor_tensor(out=sm[:, 5:6], in0=sm[:, 4:5], in1=lami[:, 0:1], op=ALU.add), reads=[sm, lami], writes=[sm])
    k.op("dve", lambda en: en.tensor_scalar(out=gdif[:], in0=gdif[:], scalar1=lami[:, 1:2], scalar2=None, op0=ALU.mult), reads=[gdif, lami], writes=[gdif])
    bidx = k.sb("bidx", [128, 1152], F32)
    k.dma("sp", bidx[:], bidxd[:, :], reads=[bidxd], writes=[bidx])
    LS = [k.sb("LS", [128, 1152], F32) for _ in range(4)]
    oh = k.sb("oh", [128, 1152], F32)
    for h in range(4):
        k.op("pool", lambda en, h=h: en.memset(LS[h][:], 0.0), writes=[LS[h]])
    for bkt in range(32 if mode == "C" else 0):
        k.op("dve", lambda en, bkt=bkt: en.tensor_scalar(out=oh[:], in0=bidx[:], scalar1=float(bkt), scalar2=None, op0=ALU.is_equal), reads=[bidx], writes=[oh])
        for h in range(4):
            k.op("dve", lambda en, h=h, bkt=bkt: en.scalar_tensor_tensor(out=LS[h][:], in0=oh[:], scalar=relb[:, bkt * 4 + h:bkt * 4 + h + 1], in1=LS[h][:], op0=ALU.mult, op1=ALU.add),
                 reads=[oh, relb, LS[h]], writes=[LS[h]])

    pS = [k.ps("pS", [128, 512], F32) for _ in range(4)]
    pO = [k.ps("pO", [128, 4, 128], F32) for _ in range(4)]
    pT = [k.sb("pT", [128, 512], BF16) for _ in range(4)]
    lg = [k.sb("lg", [128, 512], F32) for _ in range(2)]
    ost = [k.sb("ost", [128, 4, 256], F32)] * 2
    cnt = dict(s=0, t=0, o=0, l=0)

    def unit(KTb, kap_fn, qap, Vb, vslot, po, qb, bias_h=None):
        LA = 2
        pss = {}

        def qk(kt_):
            ps_ = pS[cnt["s"] % 4]
            cnt["s"] += 1
            k.op("pe", lambda en: en.matmul(ps_[:], lhsT=kap_fn(kt_), rhs=qap, start=True, stop=True), reads=[KTb[0], KTb[1]], writes=[ps_])
            pss[kt_] = ps_
        for kt in range(min(LA, NKT)):
            qk(kt)
        for kt in range(NKT):
            if kt + LA < NKT:
                qk(kt + LA)
            ps = pss.pop(kt)
            pt = pT[cnt["t"] % 4]
            cnt["t"] += 1
            if bias_h is None:
                k.op("act", lambda en: en.activation(out=pt[:], in_=ps[:], func=AF.Exp, scale=0.125), reads=[ps], writes=[pt])
            else:
                d0 = (kt - 1) - 4 * qb
                sc = 32 ** -0.5
                if kt > NQ // 128 + 1:
                    bcol = bias_h * NKT + kt
                    k.op("act", lambda en: en.activation(out=pt[:], in_=ps[:], func=AF.Exp, scale=sc, bias=tb[:, bcol:bcol + 1]), reads=[ps, tb], writes=[pt])
                elif d0 >= 5 or d0 <= -2:
                    bcol = (31 if d0 >= 5 else 15) * 4 + bias_h
                    k.op("act", lambda en: en.activation(out=pt[:], in_=ps[:], func=AF.Exp, scale=sc, bias=relb[:, bcol:bcol + 1]), reads=[ps, relb], writes=[pt])
                else:
                    l = lg[cnt["l"] % 2]
                    cnt["l"] += 1
                    c0 = (4 - d0) * 128
                    k.op("dve", lambda en: en.scalar_tensor_tensor(out=l[:], in0=ps[:], scalar=sc, in1=LS[bias_h][:, c0:c0 + 512], op0=ALU.mult, op1=ALU.add),
                         reads=[ps, LS[bias_h]], writes=[l])
                    k.op("act", lambda en: en.activation(out=pt[:], in_=l[:], func=AF.Exp), reads=[l], writes=[pt])
            for j in range(4):
                k.op("pe", lambda en, j=j: en.matmul(po[:, j, 0:65], lhsT=pt[:, j * 128:(j + 1) * 128], rhs=Vb[:, kt, vslot, :], start=(kt == 0 and j == 0), stop=(kt == NKT - 1 and j == 3), skip_group_check=True),
                     reads=[pt, Vb], writes=[(po,)])

    if mode == "A":
        KT = k.sb("KT", [128, NK], BF16)
        QT = k.sb("QT", [128, 2, NQ], BF16)
        V = k.sb("V", [128, NKT, 2, 65], BF16)
        for i4 in range(4):
            n0, n1 = (NKT * i4 // 4) * 128, (NKT * (i4 + 1) // 4) * 128
            k.dma(("sp", "act")[i4 % 2], KT[:, n0:n1], kA[:, n0:n1], reads=[kA], writes=[(KT,)])
            k.dma(("act", "sp")[i4 % 2], V[:, n0 // 128:n1 // 128], vA[n0:n1].rearrange("(t p) g d -> p t g d", p=128), reads=[vA], writes=[(V,)])
        k.dma("sp", QT[:], qA[:, :, :], reads=[qA], writes=[QT])
        for qb in range(NQB):
            os_ = ost[qb % 2]
            for h in range(4):
                g, r = h // 2, h % 2
                po = pO[cnt["o"] % 4]
                cnt["o"] += 1
                unit((KT, QT), lambda kt, g=g: KT[g * 64:(g + 1) * 64, kt * 128:(kt + 1) * 128], QT[g * 64:(g + 1) * 64, r, qb * 512:(qb + 1) * 512], V, g, po, qb)
                k.op("dve", lambda en: en.reciprocal(out=sm[:, 8:12], in_=po[:, :, 64]), reads=[po], writes=[sm])
                for j in range(4):
                    k.op("dve", lambda en, j=j: en.tensor_scalar(out=os_[:, j, h * 64:(h + 1) * 64], in0=po[:, j, 0:64], scalar1=sm[:, 8 + j:9 + j], scalar2=None, op0=ALU.mult),
                         reads=[po, sm], writes=[(os_,)])
            k.dma("sp", oa[qb * 512:(qb + 1) * 512, :].rearrange("(j p) c -> p j c", p=128), os_[:], reads=[os_], writes=[(oa,)])
    else:
        KT = k.sb("KT", [128, 2, NK], BF16)
        QT = k.sb("QT", [128, 2, 2, NQ], BF16)
        V = k.sb("V", [128, NKT, 4, 65], BF16)
        for i4 in range(4):
            n0, n1 = (NKT * i4 // 4) * 128, (NKT * (i4 + 1) // 4) * 128
            k.dma(("sp", "act")[i4 % 2], KT[:, :, n0:n1], kC[:, :, n0:n1], reads=[kC], writes=[(KT,)])
            k.dma(("act", "sp")[i4 % 2], V[:, n0 // 128:n1 // 128], vC[n0:n1].rearrange("(t p) g d -> p t g d", p=128), reads=[vC], writes=[(V,)])
        k.dma("sp", QT[:], qC[:, :, :, :], reads=[qC], writes=[QT])
        ob = k.sb("ob", [128, 64], F32)
        junk = k.sb("junk", [128, 64], F32)
        for qb in range(NQB):
            os_ = ost[qb % 2]
            for h in range(4):
                c, gi = h // 2, h % 2
                pos = []
                for m in range(2):
                    po = pO[cnt["o"] % 4]
                    cnt["o"] += 1
                    pos.append(po)
                    unit((KT, QT), lambda kt, c=c, gi=gi: KT[gi * 64:(gi + 1) * 64, c, kt * 128:(kt + 1) * 128],
                         QT[gi * 64:(gi + 1) * 64, c, m, qb * 512:(qb + 1) * 512], V, h, po, qb, bias_h=h)
                po1, po2 = pos
                k.op("dve", lambda en: en.reciprocal(out=sm[:, 8:12], in_=po1[:, :, 64]), reads=[po1], writes=[sm])
                k.op("dve", lambda en: en.reciprocal(out=sm[:, 12:16], in_=po2[:, :, 64]), reads=[po2], writes=[sm])
                k.op("dve", lambda en: en.tensor_scalar(out=sm[:, 12:16], in0=sm[:, 12:16], scalar1=sm[:, 5:6], scalar2=None, op0=ALU.mult), reads=[sm], writes=[sm])
                for j in range(4):
                    k.op("dve", lambda en, j=j: en.tensor_scalar(out=tmp[:], in0=po2[:, j, 0:64], scalar1=sm[:, 12 + j:13 + j], scalar2=None, op0=ALU.mult), reads=[po2, sm], writes=[tmp])
                    k.op("dve", lambda en, j=j: en.scalar_tensor_tensor(out=ob[:], in0=po1[:, j, 0:64], scalar=sm[:, 8 + j:9 + j], in1=tmp[:], op0=ALU.mult, op1=ALU.subtract),
                         reads=[po1, sm, tmp], writes=[ob])
                    k.op("act", lambda en: en.activation(out=junk[:], in_=ob[:], func=AF.Square, accum_out=sm[:, 6:7]), reads=[ob], writes=[junk, sm])
                    k.op("act", lambda en: en.activation(out=sm[:, 7:8], in_=sm[:, 6:7], func=AF.Sqrt, bias=eps_t[:, 0:1], scale=1.0 / 64), reads=[sm, eps_t], writes=[sm])
                    k.op("dve", lambda en: en.reciprocal(out=sm[:, 7:8], in_=sm[:, 7:8]), reads=[sm], writes=[sm])
                    k.op("dve", lambda en, j=j: en.scalar_tensor_tensor(out=os_[:, j, h * 64:(h + 1) * 64], in0=ob[:], scalar=sm[:, 7:8], in1=gdif[:], op0=ALU.mult, op1=ALU.mult),
                         reads=[ob, sm, gdif], writes=[(os_,)])
            k.dma("sp", oa[qb * 512:(qb + 1) * 512, :].rearrange("(j p) c -> p j c", p=128), os_[:], reads=[os_], writes=[(oa,)])
    return k.finish()


def c_slot_order(nkt, own0, nown, rel_bias):
    order = [own0 - 1 if own0 >= 1 else -1]
    order += list(range(own0, own0 + nown))
    order.append(own0 + nown if own0 + nown < nkt else -1)
    rest = [t for t in range(nkt) if t < own0 - 1 or t > own0 + nown]
    order += rest
    while len(order) < nkt + 1:
        order.append(-1)
    ns = nkt + 1
    tb = np.zeros((4, ns), np.float32)
    for sl, t in enumerate(order):
        if t >= 0 and sl > nown + 1:
            tb[:, sl] = rel_bias[31] if t > own0 else rel_bias[15]
    return order, tb.reshape(-1)


def c_apply_order(kC, vC, order):
    ns = len(order)
    ko = np.zeros((128, 2, ns * 128), kC.dtype)
    vo = np.zeros((ns * 128, 4, 65), vC.dtype)
    for sl, t in enumerate(order):
        if t >= 0:
            ko[:, :, sl * 128:(sl + 1) * 128] = kC[:, :, t * 128:(t + 1) * 128]
            vo[sl * 128:(sl + 1) * 128] = vC[t * 128:(t + 1) * 128]
    return ko, vo


def attn_inputs_A(paouts, c):
    b, r = c // 4, c % 4
    grp = [paouts[b * 4 + i] for i in range(4)]
    qAT = np.asarray(paouts[c]["qAT"])
    qA = np.ascontiguousarray(qAT.reshape(2, 2, 64, TQ).transpose(0, 2, 1, 3).reshape(128, 2, TQ))
    kA = np.concatenate([np.asarray(g["kAT"]) for g in grp], axis=1)
    v = np.concatenate([np.asarray(g["vA"]) for g in grp], axis=0).reshape(S, 2, 64)
    vA = np.concatenate([v, np.ones((S, 2, 1), v.dtype)], axis=-1)
    return {"qA": qA, "kA": np.ascontiguousarray(kA), "vA": np.ascontiguousarray(vA)}


def attn_inputs_C(paouts, c, inp, i):
    b, r = c // 4, c % 4
    grp = [paouts[b * 4 + i_] for i_ in range(4)]
    qCT = np.asarray(paouts[c]["qCT"])
    q4 = qCT.reshape(2, 2, 2, 32, TQ)
    qC = np.zeros((2, 64, 2, 2, TQ), qCT.dtype)
    for m in range(2):
        qC[:, m * 32:(m + 1) * 32, :, m, :] = q4[:, :, m].transpose(1, 2, 0, 3)
    qC = qC.reshape(128, 2, 2, TQ)
    kCT = np.concatenate([np.asarray(g["kCT"]) for g in grp], axis=1)
    kC = np.ascontiguousarray(kCT.reshape(2, 128, S).transpose(1, 0, 2))
    v = np.concatenate([np.asarray(g["vC"]) for g in grp], axis=0).reshape(S, 4, 64)
    vC = np.concatenate([v, np.ones((S, 4, 1), v.dtype)], axis=-1)
    order, tb = c_slot_order(S // 128, r * (TQ // 128), TQ // 128, inp["rel_bias"])
    kCs, vCs = c_apply_order(kC, vC, order)
    lam_init = 0.8 - 0.6 * math.exp(-0.3 * i)
    lamv = np.concatenate([inp["lambda_q1"][i], inp["lambda_k1"][i], inp["lambda_q2"][i], inp["lambda_k2"][i]]).astype(np.float32)
    return {"qC": np.ascontiguousarray(qC), "kC": kCs, "vC": vCs, "tb": tb,
            "bidx": _bias_bucket_strip(), "relb": np.ascontiguousarray(inp["rel_bias"].reshape(-1)),
            "lamv": lamv, "lami": np.array([lam_init, 1.0 - lam_init], np.float32), "gdiff": inp["g_diff"][i]}


def _attn_common_dummy(inp, i):
    lam_init = 0.8 - 0.6 * math.exp(-0.3 * i)
    lamv = np.concatenate([inp["lambda_q1"][i], inp["lambda_k1"][i], inp["lambda_q2"][i], inp["lambda_k2"][i]]).astype(np.float32)
    return {"bidx": _bias_bucket_strip(), "relb": np.ascontiguousarray(inp["rel_bias"].reshape(-1)),
            "lamv": lamv, "lami": np.array([lam_init, 1.0 - lam_init], np.float32), "gdiff": inp["g_diff"][i]}


def build_final(TQ=TQ):
    k = KB()
    xin = k.dram("x", [TQ, D], F32, "ExternalInput")
    gd = k.dram("g_final", [D], F32, "ExternalInput")
    yo = k.dram("y", [TQ, D], F32, "ExternalOutput")
    g = k.sb("g", [128, D], F32)
    k.dma("sp", g[:], gd[:].partition_broadcast(128), reads=[gd], writes=[g])
    eps_t = k.sb("eps", [128, 1], F32)
    k.op("dve", lambda en: en.memset(eps_t[:], EPS), writes=[eps_t])
    xt = [k.sb("xt", [128, 4, D], F32) for _ in range(2)]
    yt = [k.sb("yt", [128, 4, D], F32) for _ in range(2)]
    junk = k.sb("junk", [128, D], BF16)
    ss = k.sb("ss", [128, 4], F32)
    for blk in range(TQ // 512):
        xb, yb = xt[blk % 2], yt[blk % 2]
        t0 = blk * 512
        k.dma("sp", xb[:], xin[t0:t0 + 512, :].rearrange("(j p) d -> p j d", p=128), reads=[xin], writes=[xb])
        for j in range(4):
            k.op("act", lambda en, j=j: en.activation(out=junk[:], in_=xb[:, j, :], func=AF.Square, accum_out=ss[:, 0:1]), reads=[xb], writes=[junk, ss])
            k.op("act", lambda en: en.activation(out=ss[:, 1:2], in_=ss[:, 0:1], func=AF.Sqrt, bias=eps_t[:, 0:1], scale=1.0 / D), reads=[ss, eps_t], writes=[ss])
            k.op("dve", lambda en: en.reciprocal(out=ss[:, 2:3], in_=ss[:, 1:2]), reads=[ss], writes=[ss])
            k.op("dve", lambda en, j=j: en.scalar_tensor_tensor(out=yb[:, j, :], in0=xb[:, j, :], scalar=ss[:, 2:3], in1=g[:], op0=ALU.mult, op1=ALU.mult),
                 reads=[xb, ss, g], writes=[(yb,)])
        k.dma("act", yo[t0:t0 + 512, :].rearrange("(j p) d -> p j d", p=128), yb[:], reads=[yb], writes=[(yo,)])
    return k.finish()


def ssd_inputs(i, inp, pa, c):
    C = consts()
    b, j = c // 4, c % 4
    g = j // 2
    grp = [pa[b * 4 + r] for r in range(4)]
    xbcT = np.concatenate([np.asarray(gp["xbcT"]) for gp in grp], axis=1)
    dtT = np.concatenate([np.asarray(gp["dtT"]) for gp in grp], axis=1)
    xbc = np.zeros((128, 3, S + 4), xbcT.dtype)
    xbc[0:64, 0, 2:-2] = xbcT[j * 64:(j + 1) * 64]
    xbc[:, 1, 2:-2] = xbcT[256 + g * 128:256 + (g + 1) * 128]
    xbc[:, 2, 2:-2] = xbcT[512 + g * 128:512 + (g + 1) * 128]
    cwf = inp["conv_w"][i]
    cbf = inp["conv_b"][i]
    cw = np.zeros((128, 3, 5), np.float32)
    cb = np.zeros((128, 3), np.float32)
    cw[0:64, 0] = cwf[:, j * 64:(j + 1) * 64].T
    cw[:, 1] = cwf[:, 256 + g * 128:256 + (g + 1) * 128].T
    cw[:, 2] = cwf[:, 512 + g * 128:512 + (g + 1) * 128].T
    cb[0:64, 0] = cbf[j * 64:(j + 1) * 64]
    cb[:, 1] = cbf[256 + g * 128:256 + (g + 1) * 128]
    cb[:, 2] = cbf[512 + g * 128:512 + (g + 1) * 128]
    dtr = np.ascontiguousarray(np.stack([dtT[j], dtT[4 + j]], axis=-1).reshape(S // 128, 128, 2).transpose(1, 0, 2))
    sc = np.zeros((128, 8), np.float32)
    sc[:, 0] = inp["dt_bias_f"][i][j]
    sc[:, 1] = inp["dt_bias_b"][i][j]
    sc[:, 2] = inp["a_log_f"][i][j]
    sc[:, 3] = inp["a_log_b"][i][j]
    sc[:, 4] = inp["d_skip"][i][j]
    return {"xbc": xbc, "cw": cw, "cb": cb, "dtr": dtr, "sc": sc, "triU": C["triU"], "triL": C["triL"],
            "identf": C["identf"], "ident": C["ident"]}


def _gl(v):
    return np.ascontiguousarray(np.asarray(v).reshape(8, 128).T)


def kernel(n_layers=DEPTH, **inp):
    inp = {k_: np.asarray(v) for k_, v in inp.items()}
    C = consts()
    if "triU" not in C:
        kk = np.arange(128)
        C["triU"] = (kk[:, None] <= kk[None, :]).astype(np.float32)
        C["triL"] = (kk[:, None] >= kk[None, :]).astype(np.float32)
        C["identf"] = np.eye(128, dtype=np.float32)
        C["blk8"] = np.kron(np.eye(16), np.ones((8, 8))).astype(np.float32)
    x = inp["x"]
    xs = [np.ascontiguousarray(x[c // 4, (c % 4) * TQ:(c % 4 + 1) * TQ]) for c in range(NCORE)]
    R8 = range(NCORE)
    for i in range(n_layers):
        pa = run(prog("PA", build_PA), [pa_inputs(i, inp, c, xs) for c in R8])
        ra = run(prog("A", build_PM_attn, "A"), [dict(attn_inputs_A(pa, c), **_attn_common_dummy(inp, i)) for c in R8])
        rc = run(prog("C", build_PM_attn, "C", TQ, S + 128), [attn_inputs_C(pa, c, inp, i) for c in R8])
        rs = run(prog("SSD", build_SSD), [ssd_inputs(i, inp, pa, c) for c in R8])
        ysd = [np.ascontiguousarray(np.concatenate([np.asarray(rs[(c // 4) * 4 + j]["y"])[(c % 4) * TQ:(c % 4 + 1) * TQ] for j in range(4)], axis=1)) for c in R8]
        pbin = []
        for c in R8:
            pbin.append({"x": xs[c], "oa": np.asarray(ra[c]["o"]), "ys": ysd[c], "z": np.asarray(pa[c]["z"]), "oc": np.asarray(rc[c]["o"]),
                         "od": np.asarray(pa[c]["od"]), "w_gate": inp["w_branch_gate"][i], "w_branch": inp["w_branch"][i].reshape(1024, D),
                         "w_out": inp["w_out"][i], "w_router": np.ascontiguousarray(inp["w_router"][i].reshape(8, 128, 16).transpose(1, 0, 2)),
                         "g_mix": _gl(inp["g_mix"][i]), "g_moe": _gl(inp["g_moe"][i]), "g_ssm": inp["g_ssm"][i],
                         "ident": C["ident"], "identf": C["identf"]})
        del pa, ra, rc, rs
        pb = run(prog("PB", build_PB), pbin)
        del pbin
        thin = []
        for c in R8:
            b = c // 4
            affb = np.concatenate([np.asarray(pb[b * 4 + r]["aff"]) for r in range(4)], axis=0)
            thin.append({"affT": np.ascontiguousarray(affb.T.reshape(128, S // 8)), "blk8": C["blk8"]})
        th = run(prog("TH", build_TH), thin)
        pxin = []
        for c in R8:
            b, r = c // 4, c % 4
            pxin.append({"x1": np.asarray(pb[c]["x1"]), "aff": np.asarray(pb[c]["aff"]),
                         "thr": np.ascontiguousarray(np.asarray(th[c]["thr"]).reshape(16, 8)[:, 0]),
                         "w_exp_gate": inp["w_exp_gate"][i].reshape(16 * D, D), "w_exp_up": inp["w_exp_up"][i].reshape(16 * D, D),
                         "w_exp_down": inp["w_exp_down"][i].reshape(16 * D, D), "g_moe": _gl(inp["g_moe"][i]),
                         "p": np.ascontiguousarray(inp["p"][i][b, r * TQ:(r + 1) * TQ]), "w_ple_gate": inp["w_ple_gate"][i], "w_ple": inp["w_ple"][i],
                         "g_ple": _gl(inp["g_ple"][i]), "ident": C["ident"]})
        del pb
        px = run(prog("PX", build_PX), pxin)
        del pxin
        xs = [np.asarray(px[c]["x3"]) for c in R8]
        del px
    rf = run(prog("F", build_final), [{"x": xs[c], "g_final": inp["g_final"]} for c in R8])
    out = np.zeros((B, S, D), np.float32)
    for c in R8:
        out[c // 4, (c % 4) * TQ:(c % 4 + 1) * TQ] = np.asarray(rf[c]["y"])
    return out


def build_SSD(S_=S):
    k = KB()
    NCH = S_ // 128
    CB_ = min(2048, S_)
    xbc = k.dram("xbc", [128, 3, S_ + 4], BF16, "ExternalInput")
    cwd = k.dram("cw", [128, 3, 5], F32, "ExternalInput")
    cbd = k.dram("cb", [128, 3], F32, "ExternalInput")
    dtrd = k.dram("dtr", [128, NCH, 2], F32, "ExternalInput")
    scd = k.dram("sc", [128, 8], F32, "ExternalInput")
    triUd = k.dram("triU", [128, 128], F32, "ExternalInput")
    triLd = k.dram("triL", [128, 128], F32, "ExternalInput")
    identd = k.dram("identf", [128, 128], F32, "ExternalInput")
    identbd = k.dram("ident", [128, 128], BF16, "ExternalInput")
    yo = k.dram("y", [S_, 64], F32, "ExternalOutput")

    def ld(name, d, shape, dt):
        t = k.sb(name, shape, dt)
        k.dma("sp", t[:], d[tuple(slice(None) for _ in shape)], reads=[d], writes=[t])
        return t
    cw = ld("cw", cwd, [128, 3, 5], F32)
    cb = ld("cb", cbd, [128, 3], F32)
    dtr = ld("dtr", dtrd, [128, NCH, 2], F32)
    sc = ld("sc", scd, [128, 8], F32)
    triU = ld("triU", triUd, [128, 128], F32)
    triL = ld("triL", triLd, [128, 128], F32)
    identf = ld("identf", identd, [128, 128], F32)
    identb = ld("identb", identbd, [128, 128], BF16)
    onesf = k.sb("onesf", [128, 128], F32)
    k.op("dve", lambda en: en.memset(onesf[:], 1.0), writes=[onesf])

    XT = [k.sb("XT%d" % i, [128, S_], BF16) for i in range(3)]
    inb = [k.sb("inb", [128, 3, CB_ + 4], BF16) for _ in range(2)]
    acc = [k.sb("acc", [128, CB_], F32) for _ in range(2)]
    ai = 0
    for bi in range(S_ // CB_):
        ib = inb[bi % 2]
        c0 = bi * CB_
        k.dma("sp", ib[:], xbc[:, :, c0:c0 + CB_ + 4], reads=[xbc], writes=[ib])
        for ti in range(3):
            a = acc[ai % 2]
            ai += 1
            k.op("dve", lambda en, ti=ti: en.tensor_scalar(out=a[:], in0=ib[:, ti, 0:CB_], scalar1=cw[:, ti, 0:1], scalar2=cb[:, ti:ti + 1], op0=ALU.mult, op1=ALU.add),
                 reads=[ib, cw, cb], writes=[a])
            for tp in range(1, 5):
                k.op("dve", lambda en, ti=ti, tp=tp: en.scalar_tensor_tensor(out=a[:], in0=ib[:, ti, tp:tp + CB_], scalar=cw[:, ti, tp:tp + 1], in1=a[:], op0=ALU.mult, op1=ALU.add),
                     reads=[ib, cw, a], writes=[a])
            k.op("act", lambda en, ti=ti: en.activation(out=XT[ti][:, c0:c0 + CB_], in_=a[:], func=AF.Silu), reads=[a], writes=[(XT[ti],)])

    one_t = k.sb("one", [128, 1], F32)
    k.op("dve", lambda en: en.memset(one_t[:], 1.0), writes=[one_t])
    dt = [k.sb("dt", [128, NCH], F32) for _ in range(2)]
    dA = [k.sb("dA", [128, NCH], F32) for _ in range(2)]
    acs = [k.sb("acs", [128, NCH], F32) for _ in range(2)]
    eac = [k.sb("eac", [128, NCH], F32) for _ in range(2)]
    wv = [k.sb("wv", [128, NCH], F32) for _ in range(2)]
    cd = [k.sb("cd", [128, NCH], F32) for _ in range(2)]
    ea = k.sb("ea", [128, 2], F32)
    tmpc = k.sb("tmpc", [128, NCH], F32)
    psm = [k.ps("psm", [128, 512], F32) for _ in range(2)]
    k.op("act", lambda en: en.activation(out=ea[:], in_=sc[:, 2:4], func=AF.Exp), reads=[sc], writes=[ea])
    for d in range(2):
        k.op("act", lambda en, d=d: en.activation(out=tmpc[:], in_=dtr[:, :, d], func=AF.Exp, bias=sc[:, d:d + 1]), reads=[dtr, sc], writes=[tmpc])
        k.op("act", lambda en, d=d: en.activation(out=dt[d][:], in_=tmpc[:], func=AF.Ln, bias=one_t[:, 0:1]), reads=[tmpc, one_t], writes=[dt[d]])
        k.op("dve", lambda en, d=d: en.tensor_scalar(out=dA[d][:], in0=dt[d][:], scalar1=ea[:, d:d + 1], scalar2=-1.0, op0=ALU.mult, op1=ALU.mult),
             reads=[dt[d], ea], writes=[dA[d]])
        tri = triU if d == 0 else triL
        p1, p2 = psm
        k.op("pe", lambda en, d=d, tri=tri: en.matmul(p1[:, 0:NCH], lhsT=tri[:], rhs=dA[d][:], start=True, stop=True), reads=[tri, dA[d]], writes=[p1])
        k.op("pe", lambda en, d=d: en.matmul(p2[:, 0:NCH], lhsT=onesf[:], rhs=dA[d][:], start=True, stop=True), reads=[onesf, dA[d]], writes=[p2])
        k.op("act", lambda en, d=d: en.activation(out=acs[d][:], in_=p1[:, 0:NCH], func=AF.Copy), reads=[p1], writes=[acs[d]])
        k.op("act", lambda en, d=d: en.activation(out=eac[d][:], in_=p1[:, 0:NCH], func=AF.Exp), reads=[p1], writes=[eac[d]])
        k.op("act", lambda en, d=d: en.activation(out=cd[d][:], in_=p2[:, 0:NCH], func=AF.Exp), reads=[p2], writes=[cd[d]])
        k.op("dve", lambda en, d=d: en.tensor_tensor(out=tmpc[:], in0=p2[:, 0:NCH], in1=acs[d][:], op=ALU.subtract), reads=[p2, acs[d]], writes=[tmpc])
        k.op("act", lambda en: en.activation(out=tmpc[:], in_=tmpc[:], func=AF.Exp), reads=[tmpc], writes=[tmpc])
        k.op("dve", lambda en, d=d: en.tensor_tensor(out=wv[d][:], in0=tmpc[:], in1=dt[d][:], op=ALU.mult), reads=[tmpc, dt[d]], writes=[wv[d]])

    psR = [k.ps("psR", [128, 128], F32) for _ in range(2)]
    psCB = k.ps("psCB", [128, 128], F32)
    psT = k.ps("psT", [128, 256], BF16)
    psY = k.ps("psY", [128, 256], F32)
    y1 = k.sb("y1", [128, NCH, 64], F32)
    diag = [k.sb("diag", [128, 128], F32) for _ in range(2)]
    seg = [k.sb("seg", [128, 128], F32) for _ in range(2)]
    Wd = [k.sb("Wd", [128, 128], F32) for _ in range(2)]
    Wt = k.sb("Wt", [128, 128], F32)
    Wb16 = k.sb("Wb16", [128, 128], BF16)
    xs = k.sb("xs", [128, 64], BF16)
    Bs = k.sb("Bs", [128, 128], BF16)
    xw = k.sb("xw", [128, 64], BF16)
    run_ = [k.sb("run", [128, 64], F32) for _ in range(2)]
    prev = k.sb("prev", [128, 64], BF16)
    yst = [k.sb("yst", [128, 8, 64], F32) for _ in range(2)]
    for d in range(2):
        k.op("dve", lambda en, d=d: en.memset(run_[d][:], 0.0), writes=[run_[d]])

    def transposes(c):
        cs_ = slice(c * 128, (c + 1) * 128)
        k.op("pe", lambda en: en.transpose(out=psT[:, 0:64], in_=XT[0][0:64, cs_], identity=identb[0:64, 0:64]), reads=[XT[0], identb], writes=[(psT,)])
        k.op("pe", lambda en: en.transpose(out=psT[:, 128:256], in_=XT[1][:, cs_], identity=identb[:]), reads=[XT[1], identb], writes=[(psT,)])
        k.op("act", lambda en: en.activation(out=xs[:], in_=psT[:, 0:64], func=AF.Copy), reads=[psT], writes=[xs])
        k.op("act", lambda en: en.activation(out=Bs[:], in_=psT[:, 128:256], func=AF.Copy), reads=[psT], writes=[Bs])

    def state_step(c, d):
        cs_ = slice(c * 128, (c + 1) * 128)
        k.op("dve", lambda en: en.tensor_copy(out=prev[:], in_=run_[d][:]), reads=[run_[d]], writes=[prev])
        k.op("pe", lambda en: en.matmul(psY[:, 64:128], lhsT=XT[2][:, cs_], rhs=prev[:], start=True, stop=True), reads=[XT[2], prev], writes=[(psY,)])
        k.op("dve", lambda en: en.tensor_scalar(out=xw[:], in0=xs[:], scalar1=wv[d][:, c:c + 1], scalar2=None, op0=ALU.mult), reads=[xs, wv[d]], writes=[xw])
        k.op("pe", lambda en: en.matmul(psY[:, 128:192], lhsT=Bs[:], rhs=xw[:], start=True, stop=True), reads=[Bs, xw], writes=[(psY,)])
        k.op("dve", lambda en: en.scalar_tensor_tensor(out=run_[d][:], in0=run_[d][:], scalar=cd[d][:, c:c + 1], in1=psY[:, 128:192], op0=ALU.mult, op1=ALU.add),
             reads=[run_[d], cd[d], psY], writes=[run_[d]])

    for c in range(NCH):
        cs_ = slice(c * 128, (c + 1) * 128)
        for d in range(2):
            tri = triU if d == 0 else triL
            k.op("dve", lambda en, d=d: en.tensor_scalar(out=diag[d][:], in0=identf[:], scalar1=acs[d][:, c:c + 1], scalar2=None, op0=ALU.mult),
                 reads=[identf, acs[d]], writes=[diag[d]])
            k.op("pe", lambda en, d=d: en.matmul(psR[d][:], lhsT=onesf[:], rhs=diag[d][:], start=True, stop=True), reads=[onesf, diag[d]], writes=[psR[d]])
            k.op("dve", lambda en, d=d: en.tensor_scalar(out=seg[d][:], in0=psR[d][:], scalar1=acs[d][:, c:c + 1], scalar2=0.0, op0=ALU.subtract, op1=ALU.min),
                 reads=[psR[d], acs[d]], writes=[seg[d]])
            k.op("act", lambda en, d=d: en.activation(out=seg[d][:], in_=seg[d][:], func=AF.Exp), reads=[seg[d]], writes=[seg[d]])
            k.op("dve", lambda en, d=d, tri=tri: en.scalar_tensor_tensor(out=Wd[d][:], in0=seg[d][:], scalar=dt[d][:, c:c + 1], in1=tri[:], op0=ALU.mult, op1=ALU.mult),
                 reads=[seg[d], dt[d], tri], writes=[Wd[d]])
        k.op("pe", lambda en: en.matmul(psCB[:], lhsT=XT[1][:, cs_], rhs=XT[2][:, cs_], start=True, stop=True), reads=[XT[1], XT[2]], writes=[psCB])
        k.op("dve", lambda en: en.tensor_tensor(out=Wt[:], in0=Wd[0][:], in1=Wd[1][:], op=ALU.add), reads=[Wd[0], Wd[1]], writes=[Wt])
        k.op("dve", lambda en: en.tensor_tensor(out=Wt[:], in0=Wt[:], in1=psCB[:], op=ALU.mult), reads=[Wt, psCB], writes=[Wt])
        k.op("dve", lambda en: en.scalar_tensor_tensor(out=Wb16[:], in0=identf[:], scalar=sc[:, 4:5], in1=Wt[:], op0=ALU.mult, op1=ALU.add), reads=[identf, sc, Wt], writes=[Wb16])
        transposes(c)
        k.op("pe", lambda en: en.matmul(psY[:, 0:64], lhsT=Wb16[:], rhs=xs[:], start=True, stop=True), reads=[Wb16, xs], writes=[(psY,)])
        state_step(c, 0)
        k.op("dve", lambda en: en.tensor_copy(out=y1[:, c, :], in_=psY[:, 0:64]), reads=[psY], writes=[(y1,)])
        k.op("dve", lambda en: en.scalar_tensor_tensor(out=y1[:, c, :], in0=psY[:, 64:128], scalar=eac[0][:, c:c + 1], in1=y1[:, c, :], op0=ALU.mult, op1=ALU.add),
             reads=[psY, eac[0], y1], writes=[(y1,)])
    for c in range(NCH - 1, -1, -1):
        transposes(c)
        state_step(c, 1)
        ys = yst[(c // 8) % 2]
        k.op("dve", lambda en: en.scalar_tensor_tensor(out=ys[:, c % 8, :], in0=psY[:, 64:128], scalar=eac[1][:, c:c + 1], in1=y1[:, c, :], op0=ALU.mult, op1=ALU.add),
             reads=[psY, eac[1], y1], writes=[(ys,)])
        if c % 8 == 0:
            nb = min(8, NCH - c)
            k.dma("sp", yo[c * 128:(c + nb) * 128, :].rearrange("(j p) d -> p j d", p=128), ys[:, 0:nb, :], reads=[ys], writes=[(yo,)])
    return k.finish()


def build_PB(TQ=TQ):
    k = KB()
    NB = TQ // 512
    xin = k.dram("x", [TQ, D], F32, "ExternalInput")
    oad = k.dram("oa", [TQ, 256], F32, "ExternalInput")
    ysd = k.dram("ys", [TQ, 256], F32, "ExternalInput")
    zd = k.dram("z", [TQ, 256], F32, "ExternalInput")
    ocd = k.dram("oc", [TQ, 256], F32, "ExternalInput")
    odd = k.dram("od", [TQ, 256], BF16, "ExternalInput")
    wgd = k.dram("w_gate", [D, 4096], F32, "ExternalInput")
    wbd = k.dram("w_branch", [1024, D], F32, "ExternalInput")
    wod = k.dram("w_out", [D, D], F32, "ExternalInput")
    wrd = k.dram("w_router", [128, 8, 16], F32, "ExternalInput")
    gmixd = k.dram("g_mix", [128, 8], F32, "ExternalInput")
    gmoed = k.dram("g_moe", [128, 8], F32, "ExternalInput")
    gssmd = k.dram("g_ssm", [256], F32, "ExternalInput")
    identd = k.dram("ident", [128, 128], BF16, "ExternalInput")
    identfd = k.dram("identf", [128, 128], F32, "ExternalInput")
    x1o = k.dram("x1", [TQ, D], F32, "ExternalOutput")
    affo = k.dram("aff", [TQ, 16], F32, "ExternalOutput")

    def ld(name, d, shape, dt, bc=False):
        t = k.sb(name, shape, dt)
        src = d[:].partition_broadcast(128) if bc else d[tuple(slice(None) for _ in shape)]
        k.dma("sp", t[:], src, reads=[d], writes=[t])
        return t
    ident = ld("ident", identd, [128, 128], BF16)
    identf = ld("identf", identfd, [128, 128], F32)
    gmix = ld("gmix", gmixd, [128, 8], F32)
    gmoe = ld("gmoe", gmoed, [128, 8], F32)
    gssm = ld("gssm", gssmd, [128, 256], F32, bc=True)
    wr = ld("wr", wrd, [128, 8, 16], F32)
    for kc in range(8):
        k.op("dve", lambda en, kc=kc: en.tensor_scalar(out=wr[:, kc, :], in0=wr[:, kc, :], scalar1=gmoe[:, kc:kc + 1], scalar2=None, op0=ALU.mult), reads=[wr, gmoe], writes=[wr])
    eps_t = k.sb("eps", [128, 1], F32)
    k.op("dve", lambda en: en.memset(eps_t[:], EPS), writes=[eps_t])
    stage = [k.sb("wstage", [128, 1024], F32) for _ in range(3)]
    wg = k.sb("wg", [128, 8, 4096], BF16)
    load_cast_weight(k, wgd, 8, 4096, wg, gmix, stage)
    wb = k.sb("wb", [128, 8, 1024], BF16)
    load_cast_weight(k, wbd, 8, 1024, wb, None, stage)
    wo = k.sb("wo", [128, 8, 1024], BF16)
    load_cast_weight(k, wod, 8, 1024, wo, None, stage)

    xt = [k.sb("xt", [128, 4, D], F32) for _ in range(2)]
    hb = k.sb("hb", [128, D], BF16)
    hT = k.sb("hT", [128, 8, 512], BF16)
    brT = k.sb("brT", [128, 8, 512], BF16)
    mT = k.sb("mT", [128, 8, 512], BF16)
    macc = k.sb("macc", [128, 512], F32)
    sig = [k.sb("sig", [128, 512], F32) for _ in range(2)]
    junk = k.sb("junk", [128, D], BF16)
    ss = k.sb("ss", [128, 16], F32)
    br = k.sb("br", [128, 4, 256], BF16)
    bin_ = [k.sb("bin", [128, 4, 256], F32) for _ in range(2)]
    odt = [k.sb("odt", [128, 256], BF16) for _ in range(2)]
    t256 = k.sb("t256", [128, 256], F32)
    hmf = k.sb("hmf", [128, D], F32)
    hmT = k.sb("hmT", [128, 8, 128], F32)
    afft = [k.sb("afft", [128, 16], F32) for _ in range(2)]
    pT = [k.ps("pT", [128, D], BF16) for _ in range(2)]
    pg = [k.ps("pg", [128, 512], F32) for _ in range(2)]
    pw = [k.ps("pw", [128, 512], F32) for _ in range(2)]
    po = [k.ps("po", [128, 512], F32) for _ in range(2)]
    ci = dict(g=0, w=0, o=0, s=0)

    for blk in range(NB):
        xb = xt[blk % 2]
        t0 = blk * 512
        k.dma("sp", xb[:], xin[t0:t0 + 512, :].rearrange("(j p) d -> p j d", p=128), reads=[xin], writes=[xb])
        for j in range(4):
            r0 = t0 + j * 128
            k.op("act", lambda en, j=j: en.activation(out=junk[:], in_=xb[:, j, :], func=AF.Square, accum_out=ss[:, 0:1]), reads=[xb], writes=[junk, ss])
            k.op("act", lambda en: en.activation(out=ss[:, 1:2], in_=ss[:, 0:1], func=AF.Sqrt, bias=eps_t[:, 0:1], scale=1.0 / D), reads=[ss, eps_t], writes=[ss])
            k.op("dve", lambda en: en.reciprocal(out=ss[:, 2:3], in_=ss[:, 1:2]), reads=[ss], writes=[ss])
            k.op("dve", lambda en, j=j: en.tensor_scalar(out=hb[:], in0=xb[:, j, :], scalar1=ss[:, 2:3], scalar2=None, op0=ALU.mult), reads=[xb, ss], writes=[hb])
            pt = pT[0]
            for kc in range(8):
                k.op("pe", lambda en, kc=kc: en.transpose(out=pt[:, kc * 128:(kc + 1) * 128], in_=hb[:, kc * 128:(kc + 1) * 128], identity=ident[:]), reads=[hb, ident], writes=[(pt,)])
            k.op("act", lambda en, j=j: en.activation(out=hT[:, :, j * 128:(j + 1) * 128], in_=pt[:].rearrange("p (c t) -> p c t", c=8), func=AF.Copy), reads=[pt], writes=[(hT,)])
            bi = bin_[j % 2]
            for n_, src in enumerate((oad, ysd, zd, ocd)):
                k.dma("act", bi[:, n_, :], src[r0:r0 + 128, :], reads=[src], writes=[(bi,)])
            ot = odt[j % 2]
            k.dma("act", ot[:], odd[r0:r0 + 128, :], reads=[odd], writes=[ot])
            k.op("act", lambda en: en.activation(out=br[:, 0, :], in_=bi[:, 0, :], func=AF.Copy), reads=[bi], writes=[(br,)])
            k.op("act", lambda en: en.activation(out=br[:, 2, :], in_=bi[:, 3, :], func=AF.Copy), reads=[bi], writes=[(br,)])
            k.op("pool", lambda en: en.tensor_copy(out=br[:, 3, :], in_=ot[:]), reads=[ot], writes=[(br,)])
            k.op("act", lambda en: en.activation(out=t256[:], in_=bi[:, 2, :], func=AF.Silu), reads=[bi], writes=[t256])
            k.op("dve", lambda en: en.tensor_tensor(out=t256[:], in0=t256[:], in1=bi[:, 1, :], op=ALU.mult), reads=[t256, bi], writes=[t256])
            k.op("act", lambda en: en.activation(out=junk[:, 0:256], in_=t256[:], func=AF.Square, accum_out=ss[:, 3:4]), reads=[t256], writes=[junk, ss])
            k.op("act", lambda en: en.activation(out=ss[:, 4:5], in_=ss[:, 3:4], func=AF.Sqrt, bias=eps_t[:, 0:1], scale=1.0 / 256), reads=[ss, eps_t], writes=[ss])
            k.op("dve", lambda en: en.reciprocal(out=ss[:, 5:6], in_=ss[:, 4:5]), reads=[ss], writes=[ss])
            k.op("dve", lambda en: en.scalar_tensor_tensor(out=br[:, 1, :], in0=t256[:], scalar=ss[:, 5:6], in1=gssm[:], op0=ALU.mult, op1=ALU.mult), reads=[t256, ss, gssm], writes=[(br,)])
            pt = pT[1]
            for n_ in range(4):
                for cc in range(2):
                    q_ = n_ * 2 + cc
                    k.op("pe", lambda en, n_=n_, cc=cc, q_=q_: en.transpose(out=pt[:, q_ * 128:(q_ + 1) * 128], in_=br[:, n_, cc * 128:(cc + 1) * 128], identity=ident[:]),
                         reads=[br, ident], writes=[(pt,)])
            k.op("act", lambda en, j=j: en.activation(out=brT[:, :, j * 128:(j + 1) * 128], in_=pt[:].rearrange("p (c t) -> p c t", c=8), func=AF.Copy), reads=[pt], writes=[(brT,)])
        for dc in range(8):
            for n_ in range(4):
                g_ = pg[ci["g"] % 2]
                ci["g"] += 1
                w_ = pw[ci["w"] % 2]
                ci["w"] += 1
                col = n_ * 1024 + dc * 128
                for kc in range(8):
                    k.op("pe", lambda en, kc=kc: en.matmul(g_[:], lhsT=wg[:, kc, col:col + 128], rhs=hT[:, kc, :], start=(kc == 0), stop=(kc == 7)), reads=[wg, hT], writes=[(g_,)])
                for cc in range(2):
                    k.op("pe", lambda en, cc=cc: en.matmul(w_[:], lhsT=wb[:, n_ * 2 + cc, dc * 128:(dc + 1) * 128], rhs=brT[:, n_ * 2 + cc, :], start=(cc == 0), stop=(cc == 1)),
                         reads=[wb, brT], writes=[(w_,)])
                sg_ = sig[ci["s"] % 2]
                ci["s"] += 1
                k.op("act", lambda en: en.activation(out=sg_[:], in_=g_[:], func=AF.Sigmoid), reads=[g_], writes=[sg_])
                if n_ == 0:
                    k.op("dve", lambda en: en.tensor_tensor(out=macc[:], in0=sg_[:], in1=w_[:], op=ALU.mult), reads=[sg_, w_], writes=[macc])
                else:
                    k.op("dve", lambda en: en.tensor_tensor(out=sg_[:], in0=sg_[:], in1=w_[:], op=ALU.mult), reads=[sg_, w_], writes=[sg_])
                    if n_ < 3:
                        k.op("dve", lambda en: en.tensor_tensor(out=macc[:], in0=macc[:], in1=sg_[:], op=ALU.add), reads=[macc, sg_], writes=[macc])
                    else:
                        k.op("dve", lambda en: en.tensor_tensor(out=mT[:, dc, :], in0=macc[:], in1=sg_[:], op=ALU.add), reads=[macc, sg_], writes=[(mT,)])
        for j in range(4):
            r0 = t0 + j * 128
            for hf in range(2):
                p_ = po[ci["o"] % 2]
                ci["o"] += 1
                for dc in range(8):
                    k.op("pe", lambda en, dc=dc: en.matmul(p_[:], lhsT=mT[:, dc, j * 128:(j + 1) * 128], rhs=wo[:, dc, hf * 512:(hf + 1) * 512], start=(dc == 0), stop=(dc == 7)),
                         reads=[mT, wo], writes=[(p_,)])
                k.op("dve", lambda en, hf=hf: en.tensor_tensor(out=xb[:, j, hf * 512:(hf + 1) * 512], in0=xb[:, j, hf * 512:(hf + 1) * 512], in1=p_[:], op=ALU.add), reads=[xb, p_], writes=[(xb,)])
            k.dma("sp", x1o[r0:r0 + 128, :], xb[:, j, :], reads=[xb], writes=[(x1o,)])
            k.op("act", lambda en, j=j: en.activation(out=junk[:], in_=xb[:, j, :], func=AF.Square, accum_out=ss[:, 6:7]), reads=[xb], writes=[junk, ss])
            k.op("act", lambda en: en.activation(out=ss[:, 7:8], in_=ss[:, 6:7], func=AF.Sqrt, bias=eps_t[:, 0:1], scale=1.0 / D), reads=[ss, eps_t], writes=[ss])
            k.op("dve", lambda en: en.reciprocal(out=ss[:, 8:9], in_=ss[:, 7:8]), reads=[ss], writes=[ss])
            k.op("dve", lambda en, j=j: en.tensor_scalar(out=hmf[:], in0=xb[:, j, :], scalar1=ss[:, 8:9], scalar2=None, op0=ALU.mult), reads=[xb, ss], writes=[hmf])
            for hf in range(2):
                p_ = po[ci["o"] % 2]
                ci["o"] += 1
                for q_ in range(4):
                    kc = hf * 4 + q_
                    k.op("pe", lambda en, kc=kc, q_=q_: en.transpose(out=p_[:, q_ * 128:(q_ + 1) * 128], in_=hmf[:, kc * 128:(kc + 1) * 128], identity=identf[:]), reads=[hmf, identf], writes=[(p_,)])
                k.op("act", lambda en, hf=hf: en.activation(out=hmT[:, hf * 4:(hf + 1) * 4, :], in_=p_[:].rearrange("p (c t) -> p c t", c=4), func=AF.Copy), reads=[p_], writes=[(hmT,)])
            p_ = po[ci["o"] % 2]
            ci["o"] += 1
            for kc in range(8):
                k.op("pe", lambda en, kc=kc: en.matmul(p_[:, 0:16], lhsT=hmT[:, kc, :], rhs=wr[:, kc, :], start=(kc == 0), stop=(kc == 7)), reads=[hmT, wr], writes=[(p_,)])
            af = afft[j % 2]
            k.op("dve", lambda en: en.reduce_max(out=ss[:, 9:10], in_=p_[:, 0:16], axis=AX.X), reads=[p_], writes=[ss])
            k.op("dve", lambda en: en.tensor_scalar(out=ss[:, 10:11], in0=ss[:, 9:10], scalar1=-1.0, scalar2=None, op0=ALU.mult), reads=[ss], writes=[ss])
            k.op("act", lambda en: en.activation(out=af[:], in_=p_[:, 0:16], func=AF.Exp, bias=ss[:, 10:11], accum_out=ss[:, 11:12]), reads=[p_, ss], writes=[af, ss])
            k.op("dve", lambda en: en.reciprocal(out=ss[:, 12:13], in_=ss[:, 11:12]), reads=[ss], writes=[ss])
            k.op("dve", lambda en: en.tensor_scalar(out=af[:], in0=af[:], scalar1=ss[:, 12:13], scalar2=None, op0=ALU.mult), reads=[af, ss], writes=[af])
            k.dma("sp", affo[r0:r0 + 128, :], af[:], reads=[af], writes=[(affo,)])
    return k.finish()


def build_TH(S_=S, cap=CAP, iters=40):
    k = KB()
    W = S_ // 8
    affd = k.dram("affT", [128, W], F32, "ExternalInput")
    blkd = k.dram("blk8", [128, 128], F32, "ExternalInput")
    tho = k.dram("thr", [128, 1], F32, "ExternalOutput")
    aff = k.sb("aff", [128, W], F32)
    k.dma("sp", aff[:], affd[:, :], reads=[affd], writes=[aff])
    blk = k.sb("blk", [128, 128], F32)
    k.dma("sp", blk[:], blkd[:, :], reads=[blkd], writes=[blk])
    cmp_ = k.sb("cmp", [128, W], F32)
    v = k.sb("v", [128, 8], F32)
    ps = k.ps("ps", [128, 8], F32)
    k.op("dve", lambda en: en.memset(v[:, 0:1], 0.0), writes=[v])
    k.op("dve", lambda en: en.memset(v[:, 1:2], 1.0), writes=[v])
    for it in range(iters):
        k.op("dve", lambda en: en.tensor_tensor(out=v[:, 2:3], in0=v[:, 0:1], in1=v[:, 1:2], op=ALU.add), reads=[v], writes=[v])
        k.op("dve", lambda en: en.tensor_scalar(out=v[:, 2:3], in0=v[:, 2:3], scalar1=0.5, scalar2=None, op0=ALU.mult), reads=[v], writes=[v])
        k.op("dve", lambda en: en.tensor_scalar(out=cmp_[:], in0=aff[:], scalar1=v[:, 2:3], scalar2=None, op0=ALU.is_ge), reads=[aff, v], writes=[cmp_])
        k.op("dve", lambda en: en.reduce_sum(out=v[:, 3:4], in_=cmp_[:], axis=AX.X), reads=[cmp_], writes=[v])
        k.op("pe", lambda en: en.matmul(ps[:, 0:1], lhsT=blk[:], rhs=v[:, 3:4], start=True, stop=True), reads=[blk, v], writes=[ps])
        k.op("dve", lambda en: en.tensor_scalar(out=v[:, 4:5], in0=ps[:, 0:1], scalar1=float(cap) - 0.5, scalar2=None, op0=ALU.is_ge), reads=[ps], writes=[v])
        k.op("dve", lambda en: en.tensor_tensor(out=v[:, 5:6], in0=v[:, 2:3], in1=v[:, 0:1], op=ALU.subtract), reads=[v], writes=[v])
        k.op("dve", lambda en: en.scalar_tensor_tensor(out=v[:, 0:1], in0=v[:, 5:6], scalar=v[:, 4:5], in1=v[:, 0:1], op0=ALU.mult, op1=ALU.add), reads=[v], writes=[v])
        k.op("dve", lambda en: en.tensor_tensor(out=v[:, 5:6], in0=v[:, 1:2], in1=v[:, 2:3], op=ALU.subtract), reads=[v], writes=[v])
        k.op("dve", lambda en: en.scalar_tensor_tensor(out=v[:, 1:2], in0=v[:, 5:6], scalar=v[:, 4:5], in1=v[:, 2:3], op0=ALU.mult, op1=ALU.add), reads=[v], writes=[v])
    k.dma("sp", tho[:, :], v[:, 0:1], reads=[v], writes=[tho])
    return k.finish()


def build_PX(TQ=TQ, NE=16, TB=1024):
    k = KB()
    NT = TB // 128
    xin = k.dram("x1", [TQ, D], F32, "ExternalInput")
    affd = k.dram("aff", [TQ, 16], F32, "ExternalInput")
    thrd = k.dram("thr", [16], F32, "ExternalInput")
    wgd = k.dram("w_exp_gate", [NE * D, D], F32, "ExternalInput")
    wud = k.dram("w_exp_up", [NE * D, D], F32, "ExternalInput")
    wdd = k.dram("w_exp_down", [NE * D, D], F32, "ExternalInput")
    gmoed = k.dram("g_moe", [128, 8], F32, "ExternalInput")
    pind = k.dram("p", [TQ, 256], F32, "ExternalInput")
    wpgd = k.dram("w_ple_gate", [D, D], F32, "ExternalInput")
    wpld = k.dram("w_ple", [256, D], F32, "ExternalInput")
    gpled = k.dram("g_ple", [128, 8], F32, "ExternalInput")
    identd = k.dram("ident", [128, 128], BF16, "ExternalInput")
    xo = k.dram("x3", [TQ, D], F32, "ExternalOutput")

    ident = k.sb("ident", [128, 128], BF16)
    k.dma("sp", ident[:], identd[:, :], reads=[identd], writes=[ident])
    gmoe = k.sb("gmoe", [128, 8], F32)
    k.dma("sp", gmoe[:], gmoed[:, :], reads=[gmoed], writes=[gmoe])
    gple = k.sb("gple", [128, 8], F32)
    k.dma("sp", gple[:], gpled[:, :], reads=[gpled], writes=[gple])
    thr = k.sb("thr", [128, 16], F32)
    k.dma("sp", thr[:], thrd[:].partition_broadcast(128), reads=[thrd], writes=[thr])
    eps_t = k.sb("eps", [128, 1], F32)
    k.op("dve", lambda en: en.memset(eps_t[:], EPS), writes=[eps_t])
    estage = [k.sb("estage", [128, 1024], F32) for _ in range(3)]
    stage = estage
    wpg = k.sb("wpg", [128, 8, D], BF16)
    load_cast_weight(k, wpgd, 8, D, wpg, gple, stage)
    wpl = k.sb("wpl", [128, 2, D], BF16)
    load_cast_weight(k, wpld, 2, D, wpl, None, stage)
    Wset = [[k.sb("wg", [128, 8, D], BF16), k.sb("wu", [128, 8, D], BF16), k.sb("wd", [128, 8, D], BF16)] for _ in range(2)]

    pend = dict(pieces=[], nd=0, ncst=0)

    def pf_dma():
        i = pend["nd"]
        if i >= len(pend["pieces"]):
            return
        dram, r0, dst, kc, g = pend["pieces"][i]
        st = estage[i % 3]
        k.dma("sp", st[:], dram[r0:r0 + 128, :], reads=[dram], writes=[st])
        pend["nd"] += 1

    def pf_cast():
        i = pend["ncst"]
        if i >= len(pend["pieces"]):
            return
        dram, r0, dst, kc, g = pend["pieces"][i]
        st = estage[i % 3]
        if g is not None:
            k.op("dve", lambda en: en.tensor_scalar(out=dst[:, kc, :], in0=st[:], scalar1=g[:, kc:kc + 1], scalar2=None, op0=ALU.mult), reads=[st, g], writes=[(dst,)])
        else:
            k.op("dve", lambda en: en.tensor_copy(out=dst[:, kc, :], in_=st[:]), reads=[st], writes=[(dst,)])
        pend["ncst"] += 1

    def pf_start(e_, si):
        w3 = Wset[si]
        pieces = []
        for (dram, dst, g) in ((wgd, w3[0], gmoe), (wud, w3[1], gmoe), (wdd, w3[2], None)):
            for kc in range(8):
                pieces.append((dram, e_ * D + kc * 128, dst, kc, g))
        pend.update(pieces=pieces, nd=0, ncst=0)
        for _ in range(3):
            pf_dma()

    def pf_step():
        pf_cast()
        pf_dma()

    def pf_flush():
        while pend["ncst"] < len(pend["pieces"]):
            pf_step()
    acc = k.sb("acc", [128, NT, D], F32)
    hmT = k.sb("hmT", [128, 8, TB], BF16)
    hb = k.sb("hb", [128, D], BF16)
    junk = k.sb("junk", [128, D], BF16)
    ss = k.sb("ss", [128, 8], F32)
    aft = k.sb("aft", [128, NT, 16], F32)
    wsel = k.sb("wsel", [128, NT, 16], F32)
    hidT = k.sb("hidT", [128, 8, 512], BF16)
    sgb = [k.sb("sgb", [128, 512], F32) for _ in range(2)]
    pt_ = k.sb("pt", [128, 256], F32)
    ptb = k.sb("ptb", [128, 256], BF16)
    pTt = k.sb("pTt", [128, 2, 128], BF16)
    pT = [k.ps("pT", [128, D], BF16) for _ in range(2)]
    pg = [k.ps("pg", [128, 512], F32) for _ in range(2)]
    pu = [k.ps("pu", [128, 512], F32) for _ in range(2)]
    pd = [k.ps("pd", [128, 512], F32) for _ in range(2)]
    ci = dict(g=0, d=0, s=0, t=0)

    def norm_T(t):
        k.op("act", lambda en: en.activation(out=junk[:], in_=acc[:, t, :], func=AF.Square, accum_out=ss[:, 0:1]), reads=[acc], writes=[junk, ss])
        k.op("act", lambda en: en.activation(out=ss[:, 1:2], in_=ss[:, 0:1], func=AF.Sqrt, bias=eps_t[:, 0:1], scale=1.0 / D), reads=[ss, eps_t], writes=[ss])
        k.op("dve", lambda en: en.reciprocal(out=ss[:, 2:3], in_=ss[:, 1:2]), reads=[ss], writes=[ss])
        k.op("dve", lambda en: en.tensor_scalar(out=hb[:], in0=acc[:, t, :], scalar1=ss[:, 2:3], scalar2=None, op0=ALU.mult), reads=[acc, ss], writes=[hb])
        pt = pT[ci["t"] % 2]
        ci["t"] += 1
        for kc in range(8):
            k.op("pe", lambda en, kc=kc: en.transpose(out=pt[:, kc * 128:(kc + 1) * 128], in_=hb[:, kc * 128:(kc + 1) * 128], identity=ident[:]), reads=[hb, ident], writes=[(pt,)])
        k.op("act", lambda en: en.activation(out=hmT[:, :, t * 128:(t + 1) * 128], in_=pt[:].rearrange("p (c t) -> p c t", c=8), func=AF.Copy), reads=[pt], writes=[(hmT,)])

    for blk in range(TQ // TB):
        t0 = blk * TB
        k.dma("sp", acc[:], xin[t0:t0 + TB, :].rearrange("(j p) d -> p j d", p=128), reads=[xin], writes=[acc])
        k.dma("act", aft[:], affd[t0:t0 + TB, :].rearrange("(j p) e -> p j e", p=128), reads=[affd], writes=[aft])
        for t in range(NT):
            norm_T(t)
            k.op("dve", lambda en, t=t: en.tensor_tensor(out=wsel[:, t, :], in0=aft[:, t, :], in1=thr[:], op=ALU.is_ge), reads=[aft, thr], writes=[(wsel,)])
            k.op("dve", lambda en, t=t: en.tensor_tensor(out=wsel[:, t, :], in0=wsel[:, t, :], in1=aft[:, t, :], op=ALU.mult), reads=[wsel, aft], writes=[(wsel,)])
        for e in range(NE):
            gi_ = blk * NE + e
            if gi_ == 0:
                pf_start(0, 0)
                pf_flush()
            if gi_ + 1 < (TQ // TB) * NE:
                pf_start((e + 1) % NE, (gi_ + 1) % 2)
            wg, wu, wd = Wset[gi_ % 2]
            for sb_ in range(TB // 512):
                c0 = sb_ * 512
                for fc in range(8):
                    g_ = pg[ci["g"] % 2]
                    u_ = pu[ci["g"] % 2]
                    ci["g"] += 1
                    for kc in range(8):
                        k.op("pe", lambda en, kc=kc: en.matmul(g_[:], lhsT=wg[:, kc, fc * 128:(fc + 1) * 128], rhs=hmT[:, kc, c0:c0 + 512], start=(kc == 0), stop=(kc == 7)), reads=[wg, hmT], writes=[(g_,)])
                    for kc in range(8):
                        k.op("pe", lambda en, kc=kc: en.matmul(u_[:], lhsT=wu[:, kc, fc * 128:(fc + 1) * 128], rhs=hmT[:, kc, c0:c0 + 512], start=(kc == 0), stop=(kc == 7)), reads=[wu, hmT], writes=[(u_,)])
                    s_ = sgb[ci["s"] % 2]
                    ci["s"] += 1
                    k.op("act", lambda en: en.activation(out=s_[:], in_=g_[:], func=AF.Silu), reads=[g_], writes=[s_])
                    k.op("dve", lambda en, fc=fc: en.tensor_tensor(out=hidT[:, fc, :], in0=s_[:], in1=u_[:], op=ALU.mult), reads=[s_, u_], writes=[(hidT,)])
                    pf_step()
                for j in range(4):
                    t = sb_ * 4 + j
                    for hf in range(2):
                        d_ = pd[ci["d"] % 2]
                        ci["d"] += 1
                        for fc in range(8):
                            k.op("pe", lambda en, fc=fc: en.matmul(d_[:], lhsT=hidT[:, fc, j * 128:(j + 1) * 128], rhs=wd[:, fc, hf * 512:(hf + 1) * 512], start=(fc == 0), stop=(fc == 7)), reads=[hidT, wd], writes=[(d_,)])
                        k.op("dve", lambda en, t=t, hf=hf: en.scalar_tensor_tensor(out=acc[:, t, hf * 512:(hf + 1) * 512], in0=d_[:], scalar=wsel[:, t, e:e + 1], in1=acc[:, t, hf * 512:(hf + 1) * 512],
                                                                             op0=ALU.mult, op1=ALU.add), reads=[d_, wsel, acc], writes=[(acc,)])
                        pf_step()
            pf_flush()
        for t in range(NT):
            norm_T(t)
        for t in range(NT):
            r0 = t0 + t * 128
            k.dma("act", pt_[:], pind[r0:r0 + 128, :], reads=[pind], writes=[pt_])
            k.op("dve", lambda en: en.tensor_copy(out=ptb[:], in_=pt_[:]), reads=[pt_], writes=[ptb])
            pt = pT[ci["t"] % 2]
            ci["t"] += 1
            for cc in range(2):
                k.op("pe", lambda en, cc=cc: en.transpose(out=pt[:, cc * 128:(cc + 1) * 128], in_=ptb[:, cc * 128:(cc + 1) * 128], identity=ident[:]), reads=[ptb, ident], writes=[(pt,)])
            k.op("act", lambda en: en.activation(out=pTt[:], in_=pt[:, 0:256].rearrange("p (c t) -> p c t", c=2), func=AF.Copy), reads=[pt], writes=[pTt])
            for hf in range(2):
                g_ = pg[ci["g"] % 2]
                u_ = pu[ci["g"] % 2]
                ci["g"] += 1
                for kc in range(8):
                    k.op("pe", lambda en, kc=kc: en.matmul(g_[:], lhsT=hmT[:, kc, t * 128:(t + 1) * 128], rhs=wpg[:, kc, hf * 512:(hf + 1) * 512], start=(kc == 0), stop=(kc == 7)), reads=[hmT, wpg], writes=[(g_,)])
                for cc in range(2):
                    k.op("pe", lambda en, cc=cc: en.matmul(u_[:], lhsT=pTt[:, cc, :], rhs=wpl[:, cc, hf * 512:(hf + 1) * 512], start=(cc == 0), stop=(cc == 1)), reads=[pTt, wpl], writes=[(u_,)])
                s_ = sgb[ci["s"] % 2]
                ci["s"] += 1
                k.op("act", lambda en: en.activation(out=s_[:], in_=g_[:], func=AF.Sigmoid), reads=[g_], writes=[s_])
                k.op("dve", lambda en: en.tensor_tensor(out=s_[:], in0=s_[:], in1=u_[:], op=ALU.mult), reads=[s_, u_], writes=[s_])
                k.op("dve", lambda en, t=t, hf=hf: en.tensor_tensor(out=acc[:, t, hf * 512:(hf + 1) * 512], in0=acc[:, t, hf * 512:(hf + 1) * 512], in1=s_[:], op=ALU.add), reads=[acc, s_], writes=[(acc,)])
        k.dma("sp", xo[t0:t0 + TB, :].rearrange("(j p) d -> p j d", p=128), acc[:], reads=[acc], writes=[(xo,)])
    return k.finish()
```
